# Optimizing a Trainium2 kernel written in Bass

```python
import jax, jax.numpy as jnp
from jax import lax
import numpy as np

D_MODEL = 1024
BATCH = 32
SEQ = 2048
DEPTH = 1

N_MEM = 256
A_HEADS = 8
A_HEAD_DIM = 64
A_WIDTH = A_HEADS * A_HEAD_DIM
IDX_HEADS = 8
IDX_DIM = 64
TOPK_MAX = 256
Q_BLOCK = 128
CONV_WIDTH = 512
CONV_K = 3
X_HEADS = 4
X_HEAD_DIM = D_MODEL // X_HEADS
D_FF = 2816
ROPE_THETA = 10000.0
EPS = 1e-6

SPLITS = (A_WIDTH, A_HEAD_DIM, A_HEAD_DIM, IDX_HEADS * IDX_DIM, IDX_DIM, IDX_HEADS,
          CONV_WIDTH, CONV_WIDTH, CONV_WIDTH, D_MODEL, D_MODEL)
IN_WIDTH = (A_WIDTH + 2 * A_HEAD_DIM + IDX_HEADS * IDX_DIM + IDX_DIM + IDX_HEADS
            + 3 * CONV_WIDTH + 2 * D_MODEL)

kernel_name = "hybrid_dsa_shortconv_gated_block"


def rms_norm(x, g):
    xf = x.astype(jnp.float32)
    y = xf * lax.rsqrt(jnp.mean(xf * xf, axis=-1, keepdims=True) + EPS)
    return (y * g.astype(jnp.float32)).astype(x.dtype)


def rope_tables(seq_len, dim):
    pos = jnp.arange(seq_len, dtype=jnp.float32)
    inv = ROPE_THETA ** (-jnp.arange(0, dim, 2, dtype=jnp.float32) / dim)
    ang = pos[:, None] * inv[None, :]
    return jnp.cos(ang), jnp.sin(ang)


def apply_rope(x, cos, sin):
    xf = x.astype(jnp.float32)
    half = xf.shape[-1] // 2
    x1, x2 = xf[..., :half], xf[..., half:]
    c = cos[None, :, None, :]
    s = sin[None, :, None, :]
    return jnp.concatenate([x1 * c - x2 * s, x2 * c + x1 * s], axis=-1).astype(x.dtype)


def swiglu(x, w1, w3, w2):
    return (jax.nn.silu(x @ w1) * (x @ w3)) @ w2


def dsa_attention(q, k, v, q_idx, k_idx, w_idx):
    b, seq_len, n_heads, head_dim = q.shape
    k_top = min(TOPK_MAX, seq_len // 4)
    n_blocks = seq_len // Q_BLOCK
    key_pos = jnp.arange(seq_len)
    idx_scale = IDX_DIM ** -0.5
    attn_scale = head_dim ** -0.5

    def to_blocks(a):
        return a.reshape((b, n_blocks, Q_BLOCK) + a.shape[2:]).swapaxes(0, 1)

    def block(args):
        qb, qib, wb, t0 = args
        qpos = t0 + jnp.arange(Q_BLOCK)
        raw = jnp.einsum('bqhd,bsd->bqhs', qib, k_idx).astype(jnp.float32) * idx_scale
        score = jnp.einsum('bqhs,bqh->bqs', jax.nn.relu(raw), wb.astype(jnp.float32))
        causal = key_pos[None, :] <= qpos[:, None]
        score = jnp.where(causal[None], score, -jnp.inf)
        _, sel = lax.top_k(score, k_top)
        k_sel = jax.vmap(lambda kb, ib: kb[ib])(k, sel)
        v_sel = jax.vmap(lambda vb, ib: vb[ib])(v, sel)
        logits = jnp.einsum('bqhd,bqkd->bqhk', qb, k_sel).astype(jnp.float32) * attn_scale
        valid = (sel <= qpos[None, :, None])[:, :, None, :]
        logits = jnp.where(valid, logits, -jnp.inf)
        p = jax.nn.softmax(logits, axis=-1)
        return jnp.einsum('bqhk,bqkd->bqhd', p.astype(v.dtype), v_sel)

    outs = lax.map(block, (to_blocks(q), to_blocks(q_idx), to_blocks(w_idx),
                           jnp.arange(n_blocks) * Q_BLOCK))
    return outs.swapaxes(0, 1).reshape(b, seq_len, n_heads * head_dim)


def short_conv_causal(z, conv_w):
    c = z.shape[-1]
    kern = conv_w.reshape(CONV_K, 1, c).astype(z.dtype)
    return lax.conv_general_dilated(z, kern, window_strides=(1,), padding=[(CONV_K - 1, 0)],
                                    dimension_numbers=('NWC', 'WIO', 'NWC'),
                                    feature_group_count=c)


def memory_cross_attention(h, mem_n, w_q, w_kv, w_o):
    b, s, _ = h.shape
    m = mem_n.shape[1]
    q = (h @ w_q).reshape(b, s, X_HEADS, X_HEAD_DIM)
    kv = (mem_n @ w_kv).reshape(b, m, 2, X_HEADS, X_HEAD_DIM)
    k, v = kv[:, :, 0], kv[:, :, 1]
    logits = jnp.einsum('bshd,bmhd->bhsm', q, k).astype(jnp.float32) * (X_HEAD_DIM ** -0.5)
    p = jax.nn.softmax(logits, axis=-1)
    o = jnp.einsum('bhsm,bmhd->bshd', p.astype(v.dtype), v)
    return o.reshape(b, s, D_MODEL) @ w_o


def setup_inputs(seed: int = 0) -> dict:
    key = jax.random.key(seed)
    ks = iter(jax.random.split(key, 32))

    def w(shape, fan_in):
        return jax.random.normal(next(ks), (DEPTH,) + shape, jnp.float32) * fan_in ** -0.5

    def gain(n):
        return 1.0 + 0.05 * jax.random.normal(next(ks), (DEPTH, n), jnp.float32)

    x = jax.random.normal(next(ks), (BATCH, SEQ, D_MODEL), jnp.float32)
    mem = jax.random.normal(next(ks), (BATCH, N_MEM, D_MODEL), jnp.float32)
    return {
        "x": x,
        "mem": mem,
        "ffn1_pre_g": gain(D_MODEL),
        "ffn1_post_g": gain(D_MODEL),
        "ffn1_w1": w((D_MODEL, D_FF), D_MODEL),
        "ffn1_w3": w((D_MODEL, D_FF), D_MODEL),
        "ffn1_w2": w((D_FF, D_MODEL), D_FF),
        "mix_pre_g": gain(D_MODEL),
        "mix_post_g": gain(D_MODEL),
        "w_in": w((D_MODEL, IN_WIDTH), D_MODEL),
        "conv_w": w((CONV_K, CONV_WIDTH), CONV_K),
        "w_a_out": w((A_WIDTH, D_MODEL), A_WIDTH),
        "w_b_out": w((CONV_WIDTH, D_MODEL), CONV_WIDTH),
        "w_out": w((D_MODEL, D_MODEL), D_MODEL),
        "mem_g": gain(D_MODEL),
        "xattn_pre_g": gain(D_MODEL),
        "xattn_post_g": gain(D_MODEL),
        "xattn_w_q": w((D_MODEL, D_MODEL), D_MODEL),
        "xattn_w_kv": w((D_MODEL, 2 * D_MODEL), D_MODEL),
        "xattn_w_o": w((D_MODEL, D_MODEL), D_MODEL),
        "ffn2_pre_g": gain(D_MODEL),
        "ffn2_post_g": gain(D_MODEL),
        "ffn2_w1": w((D_MODEL, D_FF), D_MODEL),
        "ffn2_w3": w((D_MODEL, D_FF), D_MODEL),
        "ffn2_w2": w((D_FF, D_MODEL), D_FF),
    }


def reference(x, mem, ffn1_pre_g, ffn1_post_g, ffn1_w1, ffn1_w3, ffn1_w2,
              mix_pre_g, mix_post_g, w_in, conv_w, w_a_out, w_b_out, w_out,
              mem_g, xattn_pre_g, xattn_post_g, xattn_w_q, xattn_w_kv, xattn_w_o,
              ffn2_pre_g, ffn2_post_g, ffn2_w1, ffn2_w3, ffn2_w2):
    b, seq_len, _ = x.shape
    cos_a, sin_a = rope_tables(seq_len, A_HEAD_DIM)
    cos_i, sin_i = rope_tables(seq_len, IDX_DIM)
    split_points = np.cumsum(np.array(SPLITS))[:-1].tolist()
    h = x
    for l in range(DEPTH):
        f = swiglu(rms_norm(h, ffn1_pre_g[l]), ffn1_w1[l], ffn1_w3[l], ffn1_w2[l])
        h = h + 0.5 * rms_norm(f, ffn1_post_g[l])

        u = rms_norm(h, mix_pre_g[l])
        proj = u @ w_in[l]
        (q_a, k_a, v_a, q_i, k_i, w_i, gate_b, gate_c, conv_in,
         g_a, g_b) = jnp.split(proj, split_points, axis=-1)

        q_a = apply_rope(q_a.reshape(b, seq_len, A_HEADS, A_HEAD_DIM), cos_a, sin_a)
        k_a = apply_rope(k_a[:, :, None, :], cos_a, sin_a)[:, :, 0]
        q_i = apply_rope(q_i.reshape(b, seq_len, IDX_HEADS, IDX_DIM), cos_i, sin_i)
        k_i = apply_rope(k_i[:, :, None, :], cos_i, sin_i)[:, :, 0]
        w_i = w_i * (IDX_HEADS ** -0.5)
        y_a = dsa_attention(q_a, k_a, v_a, q_i, k_i, w_i) @ w_a_out[l]

        z = short_conv_causal(gate_c * conv_in, conv_w[l])
        y_b = (gate_b * z) @ w_b_out[l]

        merged = jax.nn.sigmoid(g_a) * y_a + jax.nn.sigmoid(g_b) * y_b
        h = h + rms_norm(merged @ w_out[l], mix_post_g[l])

        c = memory_cross_attention(rms_norm(h, xattn_pre_g[l]), rms_norm(mem, mem_g[l]),
                                   xattn_w_q[l], xattn_w_kv[l], xattn_w_o[l])
        h = h + rms_norm(c, xattn_post_g[l])

        f = swiglu(rms_norm(h, ffn2_pre_g[l]), ffn2_w1[l], ffn2_w3[l], ffn2_w2[l])
        h = h + 0.5 * rms_norm(f, ffn2_post_g[l])
    return h
```

```python
import numpy as np
from contextlib import ExitStack
import concourse.bass as bass
import concourse.mybir as mybir
from concourse.bass_utils import run_bass_kernel_spmd

F32 = mybir.dt.float32
BF16 = mybir.dt.bfloat16
AF = mybir.ActivationFunctionType
ALU = mybir.AluOpType
AX = mybir.AxisListType

D = 1024
DFF = 2816
NMEM = 256
EPS = 1e-6
NEG = -1.0e30
NIT = 20
EPOCH = 12000


class Buf:
    __slots__ = ("name", "lw", "rd")

    def __init__(self, name):
        self.name = name
        self.lw = None
        self.rd = []


class DmaSem:
    def __init__(self, S, name):
        self.key = ("dma", name)
        self.count = 0
        S.semkeys[self.key] = None


class Eng:
    def __init__(self, name):
        self.name = name
        self.count = 0
        self.epoch = 0
        self.ops = []
        self.known = {}


class Sched:
    def __init__(self, nc):
        self.nc = nc
        self.semkeys = {}
        self.eng = {n: Eng(n) for n in ("pe", "act", "dve", "pool", "sp")}
        for e in self.eng.values():
            self.semkeys[(e.name, 0)] = None
        self.dsems = {}
        self.nops = 0
        self.pool_deferred = []

    def buf(self, name):
        return Buf(name)

    def dmasem(self, name):
        d = DmaSem(self, name)
        self.dsems[d.key] = d
        return d

    def _deps(self, reads, writes):
        deps = []
        for b in reads:
            if b.lw is not None:
                deps.append(b.lw)
        for b in writes:
            if b.lw is not None:
                deps.append(b.lw)
            deps.extend(b.rd)
        return deps

    def _waits(self, e, deps):
        best = {}
        for key, v, snap in deps:
            if key[0] == "pe" and e.name == "pe":
                continue
            if key[0] == "dma":
                v = max(v, self.dsems[key].count)
            if e.known.get(key, 0) >= v:
                continue
            best[key] = max(best.get(key, 0), v)
            e.known[key] = v
            if snap is not None:
                for k2, v2 in snap.items():
                    if e.known.get(k2, 0) < v2:
                        e.known[k2] = v2
        return list(best.items())

    def _roll(self, e):
        if e.count >= EPOCH:
            e.epoch += 1
            e.count = 0
            self.semkeys[(e.name, e.epoch)] = None

    def op(self, eng, fn, reads=(), writes=(), signal=True):
        e = self.eng[eng]
        waits = self._waits(e, self._deps(reads, writes))
        self._roll(e)
        key = (e.name, e.epoch)
        if signal:
            e.count += 1
            tick = (key, e.count, dict(e.known))
            e.ops.append((waits, fn, key))
        else:
            tick = (key, e.count + 1, dict(e.known))
            e.ops.append((waits, fn, None))
        for b in reads:
            b.rd.append(tick)
        for b in writes:
            b.lw = tick
            b.rd = []
        self.nops += 1
        return tick

    def dma(self, eng, out, in_, sem, reads=(), writes=(), hard=False):
        e = self.eng[eng]
        waits = self._waits(e, self._deps(reads, writes))
        if hard and eng == "pool" and self.pool_deferred:
            for k, v in self.pool_deferred:
                if e.known.get(k, 0) < v:
                    waits.append((k, v))
                    e.known[k] = v
            self.pool_deferred = []
        sem.count += 16
        tick = (sem.key, sem.count, dict(e.known))

        def fn(engine, out=out, in_=in_):
            return engine.dma_start(out=out, in_=in_)
        e.ops.append((waits, fn, ("dmainc", sem.key)))
        for b in reads:
            b.rd.append(tick)
        for b in writes:
            b.lw = tick
            b.rd = []
        self.nops += 1
        return tick

    def wait_all(self, eng, bufs):
        e = self.eng[eng]
        waits = self._waits(e, self._deps((), bufs))
        e.ops.append((waits, None, None))

    def barrier(self):
        targets = []
        for e in self.eng.values():
            if e.count > 0:
                targets.append(((e.name, e.epoch), e.count))
        for key, d in self.dsems.items():
            if d.count > 0:
                targets.append((key, d.count))
        for e in self.eng.values():
            if e.name == "pool":
                self.pool_deferred = list(targets)
                continue
            waits = []
            for k, v in targets:
                if k[0] == "pe" and e.name == "pe":
                    continue
                if e.known.get(k, 0) < v:
                    waits.append((k, v))
                    e.known[k] = v
            e.ops.append((waits, None, None))

    def emit(self):
        nc = self.nc
        with ExitStack() as st:
            sems = {}
            for i, key in enumerate(self.semkeys):
                sems[key] = st.enter_context(nc.semaphore("s%d" % i))
            block = st.enter_context(nc.Block())

            def runner(e):
                def body(engine):
                    for waits, fn, key in e.ops:
                        for k, v in waits:
                            engine.wait_ge(sems[k], v)
                        if fn is None:
                            continue
                        ins = fn(engine)
                        if key is None:
                            continue
                        if key[0] == "dmainc":
                            ins.then_inc(sems[key[1]], 16)
                        else:
                            ins.then_inc(sems[key], 1)
                return body

            block.tensor(runner(self.eng["pe"]))
            block.scalar(runner(self.eng["act"]))
            block.vector(runner(self.eng["dve"]))
            block.gpsimd(runner(self.eng["pool"]))
            block.sync(runner(self.eng["sp"]))


def MM(out, lhsT, rhs, start, stop):
    return lambda e: e.matmul(out, lhsT, rhs, start=start, stop=stop)


def TR(out, in_, ident):
    return lambda e: e.transpose(out, in_, ident)


def ACT(out, in_, func, **kw):
    return lambda e: e.activation(out=out, in_=in_, func=func, **kw)


def TS(out, in0, s1, s2, op0, op1=None, **kw):
    if op1 is None:
        return lambda e: e.tensor_scalar(out=out, in0=in0, scalar1=s1, scalar2=s2, op0=op0, **kw)
    return lambda e: e.tensor_scalar(out=out, in0=in0, scalar1=s1, scalar2=s2, op0=op0, op1=op1, **kw)


def TTo(out, in0, in1, op):
    return lambda e: e.tensor_tensor(out=out, in0=in0, in1=in1, op=op)


def STT(out, in0, scalar, in1, op0, op1):
    return lambda e: e.scalar_tensor_tensor(out=out, in0=in0, scalar=scalar, in1=in1, op0=op0, op1=op1)


def CP(out, in_):
    return lambda e: e.tensor_copy(out=out, in_=in_)


def MS(ap, v):
    return lambda e: e.memset(ap, v)


_OFF = dict(qa=0, ka=512, va=576, qi=640, ki=1152, wi=1216, gb=1224, gc=1736, ci=2248, ga=2760, gbr=3784)


def _ext_cols():
    perm64 = np.concatenate([np.arange(32, 64), np.arange(0, 32)])
    cols = []
    qa = np.arange(0, 512)
    qi = np.arange(640, 1152)
    qap = (qa.reshape(8, 64)[:, perm64]).reshape(-1)
    qip = (qi.reshape(8, 64)[:, perm64]).reshape(-1)
    ka = np.arange(512, 576)
    ki = np.arange(1152, 1216)
    cols += [qa, qap, qi, qip]
    cols += [ka, ka, ka[perm64], ka[perm64], ki, ki, ki[perm64], ki[perm64]]
    vw = np.concatenate([np.arange(576, 640), np.arange(1216, 1224), np.full(56, 576)])
    cols += [vw]
    cols += [np.arange(1224, 1736), np.arange(1736, 2248), np.arange(2248, 2760)]
    cols += [np.arange(2760, 3784), np.arange(3784, 4808)]
    return np.concatenate(cols)


C_QA, C_QAP, C_QI, C_QIP = 0, 512, 1024, 1536
C_KA, C_KAP, C_KI, C_KIP = 2048, 2176, 2304, 2432
C_VW = 2560
C_GB, C_GC, C_CI = 2688, 3200, 3712
C_GA, C_GBR = 4224, 5248
C_EXT = 6272

K_G = 0
K_CW = 40
K_P2 = 52
K_TRI = 76
K_ID = 204
NCST = 332


def build(NSEQ, L, debug=False, stop=99):
    NT = L // 128
    TT = min(1024, L)
    NTT = L // TT
    CH = min(512, L)
    QH = TT
    NQH = L // QH
    KTOP = min(256, L // 4)
    TPT = TT // 128

    nc = bass.Bass("TRN2", target_bir_lowering=False)

    def din(name, shape, dt=F32):
        return nc.dram_tensor(name, list(shape), dt, kind="ExternalInput").ap()

    x_d = din("x", [NSEQ * L, D])
    mem_d = din("mem", [NSEQ * NMEM, D])
    out_d = nc.dram_tensor("out", [NSEQ * L, D], F32, kind="ExternalOutput").ap()
    w1_d = [din("f1w1", [D, DFF]), din("f2w1", [D, DFF])]
    w3_d = [din("f1w3", [D, DFF]), din("f2w3", [D, DFF])]
    w2_d = [din("f1w2", [DFF, D]), din("f2w2", [DFF, D])]
    wext_d = din("wext", [D, C_EXT])
    wa_d = din("wa", [128, 4, D])
    wb_d = din("wb", [512, D])
    wo_d = din("wo", [D, D])
    xq_d = din("xq", [D, D])
    xkv_d = din("xkv", [D, 2 * D])
    xo_d = din("xo", [D, D])
    cst_d = din("cst", [128, NCST])
    grow_d = din("grow", [4, 128, D])
    cos_d = din("ropec", [128, L])
    sin_d = din("ropes", [128, L])
    dbg_d = None
    if debug:
        dbg_d = nc.dram_tensor("dbg", [3, L, D], F32, kind="ExternalOutput").ap()

    S = Sched(nc)
    st = ExitStack()

    def sbuf(name, shape, dt):
        return st.enter_context(nc.sbuf_tensor("s_" + name, list(shape), dt))

    h = sbuf("h", [128, NT, D], F32)
    hB = [S.buf("h%d" % i) for i in range(NT)]
    AR_BYTES = 126976
    arena = sbuf("arena", [128, AR_BYTES // 2], BF16)
    cst = sbuf("cst", [128, NCST], F32)
    identb = sbuf("identb", [128, 128], BF16)
    onesb = sbuf("onesb", [128, 128], BF16)
    grow = sbuf("grow", [128, D], F32)
    stat = sbuf("stat", [128, 64], F32)
    bis = sbuf("bis", [128, 64], F32)
    junk = sbuf("junk", [128, 1024], BF16)
    hn = sbuf("hn", [128, 1024], BF16)
    Bhn = S.buf("hn")
    Bcst, Bid, Bones, Bgrow, Bjunk = (S.buf(n) for n in ("cst", "id", "ones", "grow", "junk"))
    Bjunk2 = S.buf("junk2")
    halo = sbuf("halo", [128, 8], F32)
    Bhalo = S.buf("halo")

    def carve(off, free_shape, dt):
        n = int(np.prod(free_shape))
        if dt == BF16:
            ap = arena[:, off // 2: off // 2 + n]
        else:
            ap = arena[:, off // 2: off // 2 + 2 * n].bitcast(F32)
        if len(free_shape) == 2:
            ap = ap.rearrange("p (a b) -> p a b", a=free_shape[0])
        elif len(free_shape) == 3:
            ap = ap.rearrange("p (a b c) -> p a b c", a=free_shape[0], b=free_shape[1])
        return ap

    POOL_OFF = 106496
    NSLOT = 5
    slots = []
    for i in range(NSLOT):
        slots.append(dict(off=POOL_OFF + i * 4096, buf=S.buf("slot%d" % i), sem=S.dmasem("slot%d" % i)))
    slot_ctr = [0]

    def wload(dview, a, b):
        assert a * b <= 2048, (a, b)
        sl = slots[slot_ctr[0] % NSLOT]
        slot_ctr[0] += 1
        t = carve(sl["off"], [a, b], BF16)
        S.dma("pool", t, dview, sl["sem"], writes=[sl["buf"]])
        return t, sl["buf"]

    psb = []
    for i in range(8):
        t = st.enter_context(nc.psum_tensor("ps%d" % i, [128, 512], F32))
        psb.append((t, S.buf("ps%d" % i)))
    rot = [0]

    def bank(lo=0, hi=4):
        i = lo + rot[0] % (hi - lo)
        rot[0] += 1
        return psb[i]

    sem_c = S.dmasem("const")
    S.dma("sp", cst[:], cst_d, sem_c, writes=[Bcst])
    S.dma("pool", identb[:], cst_d[:, K_ID:K_ID + 128], S.dmasem("const2"), writes=[Bid])
    S.op("dve", MS(onesb[:], 1.0), writes=[Bones])
    tri = cst[:, K_TRI:K_TRI + 128]
    identf = cst[:, K_ID:K_ID + 128]
    sem_g = S.dmasem("grow")
    sem_io = S.dmasem("io")
    sem_out = S.dmasem("out")
    sem_rope = S.dmasem("rope")

    stat_ctr = [0]
    Bstat = [S.buf("stat%d" % i) for i in range(16)]
    S.op("dve", MS(stat[:], 0.0), writes=Bstat)

    def stat_slot():
        i = stat_ctr[0] % 16
        stat_ctr[0] += 1
        return stat[:, 4 * i:4 * i + 4], Bstat[i]

    def rstd_from_ss(ss_ap, ssB, half):
        sl, slB = stat_slot()
        c = 4.0 if half else 1.0
        S.op("act", ACT(sl[:, 0:1], ss_ap, AF.Sqrt, scale=c / D, bias=c * EPS), reads=[ssB], writes=[slB])
        S.op("dve", lambda e: e.reciprocal(out=sl[:, 1:2], in_=sl[:, 0:1]), reads=[slB], writes=[slB])
        return sl[:, 1:2], slB

    def prenorm_T(src_tile, srcB, gidx, dst, dstB_of, ncols_dst0, ntiles):
        for i in range(ntiles):
            sl, slB = stat_slot()
            src = src_tile(i)
            S.op("act", ACT(junk[:], src, AF.Square, accum_out=sl[:, 2:3]), reads=[srcB(i)], writes=[Bjunk, slB])
            rs, rsB = rstd_from_ss(sl[:, 2:3], slB, False)
            S.op("act", ACT(hn[:], src, AF.Copy, scale=rs), reads=[srcB(i), rsB], writes=[Bhn])
            pt, pB = bank(0, 4)
            ptb = pt[:].bitcast(BF16)
            for dc in range(8):
                S.op("pe", TR(ptb[:, dc * 128:(dc + 1) * 128], hn[:, dc * 128:(dc + 1) * 128], identb[:]),
                     reads=[Bhn, Bid], writes=[pB], signal=(dc == 7))
            gv = cst[:, K_G + gidx * 8:K_G + gidx * 8 + 8]
            S.op("dve", TTo(dst[:, :, ncols_dst0 + i * 128: ncols_dst0 + (i + 1) * 128],
                            ptb.rearrange("p (a b) -> p a b", a=8),
                            gv.unsqueeze(2).to_broadcast([128, 8, 128]), ALU.mult),
                 reads=[pB, Bcst], writes=[dstB_of(i)])

    def postnorm_res(pa, paB, pb, pbB, ti, half):
        sl, slB = stat_slot()
        S.op("act", ACT(junk[:, 0:512], pa, AF.Square, accum_out=sl[:, 0:1]), reads=[paB], writes=[Bjunk, slB])
        S.op("act", ACT(junk[:, 512:1024], pb, AF.Square, accum_out=sl[:, 1:2]), reads=[pbB], writes=[Bjunk, slB])
        S.op("dve", TTo(sl[:, 2:3], sl[:, 0:1], sl[:, 1:2], ALU.add), reads=[slB], writes=[slB])
        rs, rsB = rstd_from_ss(sl[:, 2:3], slB, half)
        for k, (pp, ppB) in enumerate(((pa, paB), (pb, pbB))):
            hs = h[:, ti, k * 512:(k + 1) * 512]
            tmp = resid_tmp[k]
            S.op("dve", STT(tmp, pp, rs, grow[:, k * 512:(k + 1) * 512], ALU.mult, ALU.mult),
                 reads=[ppB, rsB, Bgrow], writes=[resid_B[k]])
            S.op("dve", TTo(hs, hs, tmp, ALU.add), reads=[resid_B[k], hB[ti]], writes=[hB[ti]])

    aux = sbuf("aux", [128, 2048], F32)
    resid_tmp = [aux[:, 1024:1536], aux[:, 1536:2048]]
    resid_B = [S.buf("resid0"), S.buf("resid1")]

    def load_grow(idx):
        S.dma("sp", grow[:], grow_d[idx], sem_g, writes=[Bgrow])

    uT_full = carve(0, [8, L], BF16)
    uTB = [S.buf("uT%d" % i) for i in range(NT)]

    def ffn(seq, tt, which, gidx_pre, grow_idx):
        t0 = tt * TT
        nch = TT // CH
        uTf = carve(0, [8, TT], BF16)
        gT = carve(16384, [22, TT], BF16)
        gTB = [S.buf("gT%d" % f) for f in range(22)]
        W2s = carve(61440, [22, D], BF16)
        W2B = S.buf("W2s")
        load_grow(grow_idx)
        prenorm_T(lambda i: h[:, tt * TPT + i, :], lambda i: hB[tt * TPT + i], gidx_pre,
                  uTf, lambda i: uTB[i], 0, TPT)
        w2v = w2_d[which].rearrange("(fc p) d -> p fc d", p=128)
        sem_w2 = sem_w2_list[0]
        w1v = w1_d[which].rearrange("(kc p) f -> p kc f", p=128)
        w3v = w3_d[which].rearrange("(kc p) f -> p kc f", p=128)
        for u in range(11):
            wt1, wb1 = wload(w1v[:, :, u * 256:(u + 1) * 256], 8, 256)
            wt3, wb3 = wload(w3v[:, :, u * 256:(u + 1) * 256], 8, 256)
            if u == 1:
                for f in range(0, 22, 2):
                    S.dma("pool", W2s[:, f:f + 2, :], w2v[:, f:f + 2, :], sem_w2, writes=[W2B], hard=True)
            for j in range(2):
                f = u * 2 + j
                for c in range(nch):
                    pg, pgB = bank(0, 4)
                    pu, puB = bank(0, 4)
                    rB = [uTB[(c * CH) // 128 + q] for q in range(CH // 128)]
                    for kc in range(8):
                        S.op("pe", MM(pg[:, 0:CH], wt1[:, kc, j * 128:(j + 1) * 128], uTf[:, kc, c * CH:(c + 1) * CH],
                                      kc == 0, kc == 7), reads=[wb1] + rB, writes=[pgB], signal=(kc == 7))
                    for kc in range(8):
                        S.op("pe", MM(pu[:, 0:CH], wt3[:, kc, j * 128:(j + 1) * 128], uTf[:, kc, c * CH:(c + 1) * CH],
                                      kc == 0, kc == 7), reads=[wb3] + rB, writes=[puB], signal=(kc == 7))
                    sg = silu_t[rot[0] % 2]
                    sgB = silu_B[rot[0] % 2]
                    S.op("act", ACT(sg[:, 0:CH], pg[:, 0:CH], AF.Silu), reads=[pgB], writes=[sgB])
                    S.op("dve", TTo(gT[:, f, c * CH:(c + 1) * CH], pu[:, 0:CH], sg[:, 0:CH], ALU.mult),
                         reads=[puB, sgB], writes=[gTB[f]])
        for i in range(TPT):
            pa, paB = bank(4, 8)
            pb, pbB = bank(4, 8)
            for k, (pp, ppB) in enumerate(((pa, paB), (pb, pbB))):
                for f in range(22):
                    S.op("pe", MM(pp[:, :], gT[:, f, i * 128:(i + 1) * 128], W2s[:, f, k * 512:(k + 1) * 512],
                                  f == 0, f == 21), reads=[gTB[f], W2B], writes=[ppB], signal=(f == 21))
            postnorm_res(pa[:, :], paB, pb[:, :], pbB, tt * TPT + i, True)

    sem_w2_list = [S.dmasem("w2")]
    silu_t = [aux[:, 0:512], aux[:, 512:1024]]
    silu_B = [S.buf("silu0"), S.buf("silu1")]

    def fm_linear(wv, c0, ncol, xT, xB_of_chunk, t0, ntok, evac, kcn=8, unit=256, CH=CH):
        unit = min(unit, 2048 // kcn)
        for u0 in range(0, ncol, unit):
            un = min(unit, ncol - u0)
            wt, wB = wload(wv[:, :, c0 + u0:c0 + u0 + un], kcn, un)
            for j in range(0, un, 128):
                jn = min(128, un - j)
                for tc in range(0, ntok, CH):
                    pt, pB = bank(0, 4)
                    for kc in range(kcn):
                        S.op("pe", MM(pt[0:jn, 0:CH], wt[:, kc, j:j + jn], xT[:, kc, t0 + tc:t0 + tc + CH],
                                      kc == 0, kc == kcn - 1),
                             reads=[wB] + xB_of_chunk(t0 + tc), writes=[pB], signal=(kc == kcn - 1))
                    evac((u0 + j) // 128, tc, pt, pB, jn)

    MP = 32768
    qaT = carve(MP, [4, QH], BF16)
    qiT = carve(MP + 8192, [4, QH], BF16)
    kaT = carve(MP + 16384, [L], BF16)
    kiT = carve(MP + 20480, [L], BF16)
    vS = carve(MP + 24576, [NT, 64], BF16)
    wiS = carve(MP + 26624, [NT, 8], F32)
    zgT = carve(MP + 27136, [4, QH], BF16)
    oT = carve(MP + 35328, [4, QH], BF16)
    RX = MP + 43520
    ropeC = carve(RX, [CH], F32)
    ropeS = carve(RX + 2048, [CH], F32)
    rt1 = carve(RX + 4096, [CH], F32)
    rt2 = carve(RX + 6144, [CH], F32)
    ybuf = carve(RX + 8192, [4, CH + 2], F32)
    ctmp = carve(RX + 8192 + 8224, [CH], F32)
    gcs = carve(RX + 8192 + 8224 + 2048, [CH], F32)
    sc = carve(RX, [L], F32)
    maskS = carve(RX + 8192, [L], BF16)
    maskT = carve(RX + 12288, [NT, 128], BF16)
    rh = [carve(RX + 16384, [512], F32), carve(RX + 18432, [512], F32)]
    PTs = [carve(RX + 20480 + i * 1024, [4, 128], BF16) for i in range(3)]
    rdn = carve(RX + 23552, [4, 128], F32)
    mask2 = carve(RX + 25600, [L], BF16)
    mrgT = carve(RX, [8, QH], BF16)
    sgm = carve(RX + 16384, [CH], F32)
    tmpA = carve(RX + 18432, [CH], F32)
    tmpB = carve(RX + 20480, [CH], F32)

    wev = wext_d.rearrange("(kc p) c -> p kc c", p=128)

    def mixer(seq):
        nqb_h = QH // 128
        BqaT = [S.buf("qaT%d" % i) for i in range(QH // CH)]
        BqiT = [S.buf("qiT%d" % i) for i in range(QH // CH)]
        BkaT = [S.buf("kaT%d" % i) for i in range(L // CH)]
        BkiT = [S.buf("kiT%d" % i) for i in range(L // CH)]
        BvS = [S.buf("vS%d" % i) for i in range(NT)]
        BwiS = [S.buf("wiS%d" % i) for i in range(NT)]
        BzgT = [S.buf("zgT%d" % i) for i in range(QH // CH)]
        BoT = [S.buf("oT%d" % i) for i in range(nqb_h)]
        BropeC, BropeS, Brt1, Brt2 = (S.buf(n) for n in ("ropeC", "ropeS", "rt1", "rt2"))
        Bybuf, Bctmp, Bgcs = (S.buf(n) for n in ("ybuf", "ctmp", "gcs"))
        Bsc, Bmask, BmaskT = S.buf("sc"), S.buf("mask"), S.buf("maskT")
        Brh = [S.buf("rh0"), S.buf("rh1")]
        BPT = [S.buf("PT%d" % i) for i in range(3)]
        Bmrg = [S.buf("mrg%d" % i) for i in range(QH // CH)]
        Bsgm, BtmpA, BtmpB = S.buf("sgm"), S.buf("tmpA"), S.buf("tmpB")
        Bbis = S.buf("bis")

        def xB(tcol):
            return [uTB[tcol // 128 + q] for q in range(CH // 128)]

        prenorm_T(lambda i: h[:, i, :], lambda i: hB[i], 1, uT_full, lambda i: uTB[i], 0, NT)

        def load_rope(tcol):
            S.dma("sp", ropeC[:], cos_d[:, tcol:tcol + CH], sem_rope, writes=[BropeC])
            S.dma("sp", ropeS[:], sin_d[:, tcol:tcol + CH], sem_rope, writes=[BropeS])

        def roped(c_plain, c_perm, ncol128, dst_of, dstB_of, t0, ntok):
            for g in range(ncol128):
                wt, wB = wload(wev[:, :, c_plain + g * 128:c_plain + (g + 1) * 128], 8, 128)
                wp, wpB = wload(wev[:, :, c_perm + g * 128:c_perm + (g + 1) * 128], 8, 128)
                for tc in range(0, ntok, CH):
                    p1, p1B = bank(0, 4)
                    p2, p2B = bank(0, 4)
                    for kc in range(8):
                        S.op("pe", MM(p1[:, 0:CH], wt[:, kc, :], uT_full[:, kc, t0 + tc:t0 + tc + CH], kc == 0, kc == 7),
                             reads=[wB] + xB(t0 + tc), writes=[p1B], signal=(kc == 7))
                    for kc in range(8):
                        S.op("pe", MM(p2[:, 0:CH], wp[:, kc, :], uT_full[:, kc, t0 + tc:t0 + tc + CH], kc == 0, kc == 7),
                             reads=[wpB] + xB(t0 + tc), writes=[p2B], signal=(kc == 7))
                    yield_rope(t0 + tc)
                    S.op("dve", TTo(rt1[:], p1[:, 0:CH], ropeC[:], ALU.mult), reads=[p1B, BropeC], writes=[Brt1])
                    S.op("dve", TTo(rt2[:], p2[:, 0:CH], ropeS[:], ALU.mult), reads=[p2B, BropeS], writes=[Brt2])
                    S.op("dve", TTo(dst_of(g, tc), rt1[:], rt2[:], ALU.add), reads=[Brt1, Brt2],
                         writes=[dstB_of(g, tc)])

        rope_state = [None]

        def yield_rope(tcol):
            if rope_state[0] != tcol:
                load_rope(tcol)
                rope_state[0] = tcol

        for qh in range(NQH):
            q0 = qh * QH
            rope_state[0] = None
            if qh == 0:
                for tc in range(0, L, CH):
                    roped(C_KA, C_KAP, 1, lambda g, t, tc=tc: kaT[:, tc:tc + CH], lambda g, t, tc=tc: BkaT[tc // CH], tc, CH)
                    roped(C_KI, C_KIP, 1, lambda g, t, tc=tc: kiT[:, tc:tc + CH], lambda g, t, tc=tc: BkiT[tc // CH], tc, CH)
                wt, wB = wload(wev[:, :, C_VW:C_VW + 128], 8, 128)
                for i in range(NT):
                    pt, pB = bank(0, 4)
                    for kc in range(8):
                        S.op("pe", MM(pt[:, 0:72], uT_full[:, kc, i * 128:(i + 1) * 128], wt[:, kc, 0:72], kc == 0, kc == 7),
                             reads=[wB, uTB[i]], writes=[pB], signal=(kc == 7))
                    S.op("act", ACT(vS[:, i, :], pt[:, 0:64], AF.Copy), reads=[pB], writes=[BvS[i]])
                    S.op("dve", TS(wiS[:, i, :], pt[:, 64:72], float(8 ** -0.5 * 64 ** -0.5), None, ALU.mult),
                         reads=[pB], writes=[BwiS[i]])
            for tc in range(0, QH, CH):
                roped(C_QA, C_QAP, 4, lambda g, t, tc=tc: qaT[:, g, tc:tc + CH], lambda g, t, tc=tc: BqaT[tc // CH], q0 + tc, CH)
                roped(C_QI, C_QIP, 4, lambda g, t, tc=tc: qiT[:, g, tc:tc + CH], lambda g, t, tc=tc: BqiT[tc // CH], q0 + tc, CH)
            cw = cst[:, K_CW:K_CW + 12].rearrange("p (g k) -> p g k", g=4)
            for tc in range(0, QH, CH):
                first = (q0 + tc == 0)
                for g in range(4):
                    if first:
                        S.op("dve", MS(ybuf[:, g, 0:2], 0.0), writes=[Bybuf])
                    elif tc == 0:
                        S.op("dve", CP(ybuf[:, g, 0:2], halo[:, 2 * g:2 * g + 2]), reads=[Bhalo], writes=[Bybuf])
                    else:
                        S.op("dve", CP(ybuf[:, g, 0:2], ybuf[:, g, CH:CH + 2]), reads=[Bybuf], writes=[Bybuf])

                    def ev_gc(ci, t, pt, pB, jn):
                        S.op("act", ACT(gcs[:], pt[:, 0:CH], AF.Copy), reads=[pB], writes=[Bgcs])
                    fm_linear(wev, C_GC + g * 128, 128, uT_full, xB, q0 + tc, CH, ev_gc, unit=128)

                    def ev_ci(ci, t, pt, pB, jn, g=g):
                        S.op("dve", TTo(ybuf[:, g, 2:CH + 2], pt[:, 0:CH], gcs[:], ALU.mult), reads=[pB, Bgcs], writes=[Bybuf])
                        S.op("dve", TS(ctmp[:], ybuf[:, g, 2:CH + 2], cw[:, g, 2:3], None, ALU.mult),
                             reads=[Bybuf, Bcst], writes=[Bctmp])
                        S.op("dve", STT(ctmp[:], ybuf[:, g, 1:CH + 1], cw[:, g, 1:2], ctmp[:], ALU.mult, ALU.add),
                             reads=[Bybuf, Bcst, Bctmp], writes=[Bctmp])
                        S.op("dve", STT(ctmp[:], ybuf[:, g, 0:CH], cw[:, g, 0:1], ctmp[:], ALU.mult, ALU.add),
                             reads=[Bybuf, Bcst, Bctmp], writes=[Bctmp])
                    fm_linear(wev, C_CI + g * 128, 128, uT_full, xB, q0 + tc, CH, ev_ci, unit=128)

                    def ev_gb(ci, t, pt, pB, jn, g=g, tc=tc):
                        S.op("dve", TTo(zgT[:, g, tc:tc + CH], pt[:, 0:CH], ctmp[:], ALU.mult), reads=[pB, Bctmp],
                             writes=[BzgT[tc // CH]])
                    fm_linear(wev, C_GB + g * 128, 128, uT_full, xB, q0 + tc, CH, ev_gb, unit=128)
                    if tc + CH == QH and qh + 1 < NQH:
                        S.op("dve", CP(halo[:, 2 * g:2 * g + 2], ybuf[:, g, CH:CH + 2]), reads=[Bybuf], writes=[Bhalo])
            S.barrier()
            if stop < 4:
                continue

            p2 = cst[:, K_P2:K_P2 + NIT + 2]
            scs = [sc, aux[:, 0:L]]
            Bscs = [S.buf("sc0"), S.buf("sc1")]
            masks = [maskS, mask2]
            BmL = [S.buf("mL0"), S.buf("mL1")]
            BmR = [S.buf("mR0"), S.buf("mR1")]
            Bb = [dict((n, S.buf("b%s%d" % (n, p))) for n in ("mid", "cd", "sa", "tot", "tmp", "wt")) for p in range(2)]

            def stage_I(qb):
                gq = q0 // 128 + qb
                nk = (gq + 1) * 128
                tq = slice(qb * 128, (qb + 1) * 128)
                p = qb % 2
                for k0 in range(0, nk, 512):
                    kw = min(512, nk - k0)
                    pacc, paccB = psb[2]
                    for hh in range(8):
                        pr, prB = psb[hh % 2]
                        pb_ = (hh % 2) * 64
                        S.op("pe", MM(pr[:, 0:kw], qiT[pb_:pb_ + 64, hh // 2, tq], kiT[pb_:pb_ + 64, k0:k0 + kw], True, True),
                             reads=[BqiT[(qb * 128) // CH], BkiT[k0 // CH], BkiT[(k0 + kw - 1) // CH]], writes=[prB])
                        r, rB = rh[hh % 2], Brh[hh % 2]
                        S.op("act", ACT(r[:, 0:kw], pr[:, 0:kw], AF.Relu), reads=[prB], writes=[rB])
                        wcol = wiS[:, gq, hh:hh + 1]
                        if hh == 0:
                            S.op("dve", TS(pacc[:, 0:kw], r[:, 0:kw], wcol, None, ALU.mult), reads=[rB, BwiS[gq]],
                                 writes=[paccB])
                        else:
                            S.op("dve", STT(pacc[:, 0:kw], r[:, 0:kw], wcol, pacc[:, 0:kw], ALU.mult, ALU.add),
                                 reads=[rB, BwiS[gq], paccB], writes=[paccB])
                        yield
                    S.op("act", ACT(scs[p][:, k0:k0 + kw], pacc[:, 0:kw], AF.Copy), reads=[paccB], writes=[Bscs[p]])

            def stage_B(qb):
                gq = q0 // 128 + qb
                nk = (gq + 1) * 128
                p = qb % 2
                sc_, Bsc_ = scs[p], Bscs[p]
                mk = masks[p]
                bb = Bb[p]
                o = 32 * p
                cA, cmid, ccd, csa, ctot, ctmp_, cthr, cW = (bis[:, o + i:o + i + 1] for i in range(8))
                wt_ = bis[:, o + 8:o + 8 + NIT + 2]
                a = 0 if nk <= 256 else int(round(0.3 * nk / 64.0)) * 64
                n_act = nk - a
                thrc = float(KTOP) - 0.5 - 0.5 * n_act
                S.op("dve", lambda e: e.tensor_reduce(out=cA, in_=sc_[:, 0:nk], axis=AX.X, op=ALU.max,
                                                       apply_absolute_value=True), reads=[Bsc_], writes=[bb["wt"]])
                S.op("dve", TS(cW, cA, 2.000002, 1.0e-20, ALU.mult, ALU.add), reads=[bb["wt"]], writes=[bb["wt"]])
                S.op("dve", TS(wt_, p2, cW, None, ALU.mult), reads=[bb["wt"], Bcst], writes=[bb["wt"]])
                S.op("dve", TTo(sc_[:, nk - 128:nk], sc_[:, nk - 128:nk], tri, ALU.add), reads=[Bsc_, Bcst], writes=[Bsc_])
                S.op("dve", MS(cmid, 0.0), writes=[bb["mid"]])
                S.op("dve", MS(ccd, 0.0), writes=[bb["cd"]])
                yield
                for it in range(1, NIT + 1):
                    S.op("act", ACT(mk[:, a:nk], sc_[:, a:nk], AF.Sign, scale=-1.0, bias=cmid, accum_out=csa),
                         reads=[Bsc_, bb["mid"]], writes=[BmR[p], bb["sa"]])
                    if a > 0:
                        S.op("dve", TS(mk[:, 0:a], sc_[:, 0:a], cmid, None, ALU.is_ge, ALU.add, accum_out=ccd),
                             reads=[Bsc_, bb["mid"]], writes=[BmL[p], bb["cd"]])
                    yield
                    S.op("dve", STT(ctot, csa, -0.5, ccd, ALU.mult, ALU.add), reads=[bb["sa"], bb["cd"]], writes=[bb["tot"]])
                    S.op("dve", TS(ctmp_, ctot, thrc, wt_[:, it:it + 1], ALU.is_ge, ALU.mult), reads=[bb["tot"], bb["wt"]],
                         writes=[bb["tmp"]])
                    if it < NIT:
                        S.op("dve", TS(cmid, cmid, wt_[:, it + 1:it + 2], ctmp_, ALU.subtract, ALU.add),
                             reads=[bb["mid"], bb["tmp"], bb["wt"]], writes=[bb["mid"]])
                    else:
                        S.op("dve", TS(cthr, cmid, wt_[:, it:it + 1], ctmp_, ALU.subtract, ALU.add),
                             reads=[bb["mid"], bb["tmp"], bb["wt"]], writes=[bb["mid"]])
                    yield
                S.op("dve", TS(mk[:, 0:nk], sc_[:, 0:nk], cthr, None, ALU.is_ge), reads=[Bsc_, bb["mid"]],
                     writes=[BmL[p], BmR[p]])
                yield

            def stage_A(qb):
                gq = q0 // 128 + qb
                tq = slice(qb * 128, (qb + 1) * 128)
                p = qb % 2
                mk = masks[p]
                pm, pmB = psb[3]
                pmb = pm[:].bitcast(BF16)
                for j0 in range(0, gq + 1, 8):
                    jn = min(8, gq + 1 - j0)
                    for j in range(jn):
                        S.op("pe", TR(pmb[:, j * 128:(j + 1) * 128], mk[:, (j0 + j) * 128:(j0 + j + 1) * 128], identb[:]),
                             reads=[BmL[p], BmR[p], Bid], writes=[pmB], signal=(j == jn - 1))
                    S.op("act", ACT(maskT[:, j0:j0 + jn, :], pmb[:, 0:jn * 128].rearrange("p (a b) -> p a b", a=jn), AF.Copy),
                         reads=[pmB], writes=[BmaskT])
                yield
                pX, pXB = psb[6]
                pY, pYB = psb[7]
                ctr = 0
                for j in range(gq + 1):
                    ks = slice(j * 128, (j + 1) * 128)
                    for pair in range(4):
                        for par in range(2):
                            pl, plB = psb[4 + par]
                            pb_ = par * 64
                            S.op("pe", MM(pl[:, pair * 128:(pair + 1) * 128], kaT[pb_:pb_ + 64, ks], qaT[pb_:pb_ + 64, pair, tq],
                                          True, True), reads=[BkaT[(j * 128) // CH], BqaT[(qb * 128) // CH]], writes=[plB],
                                 signal=(pair == 3 and par == 1))
                    for par in range(2):
                        pl, plB = psb[4 + par]
                        PT, PTB = PTs[ctr % 3], BPT[ctr % 3]
                        ctr += 1
                        S.op("act", ACT(PT[:], pl[:].rearrange("p (a b) -> p a b", a=4), AF.Exp, scale=0.125),
                             reads=[plB], writes=[PTB])
                        S.op("dve", TTo(PT[:], PT[:], maskT[:, j, :].unsqueeze(1).to_broadcast([128, 4, 128]), ALU.mult),
                             reads=[PTB, BmaskT], writes=[PTB])
                        ptf = PT[:].rearrange("p a b -> p (a b)")
                        S.op("pe", MM(pX[par * 64:(par + 1) * 64, :], vS[:, j, :], ptf, j == 0, j == gq),
                             reads=[BvS[j], PTB], writes=[pXB], signal=False)
                        S.op("pe", MM(pY[par * 64:(par + 1) * 64, :], onesb[:, 0:64], ptf, j == 0, j == gq),
                             reads=[Bones, PTB], writes=[pYB], signal=True)
                    yield
                S.op("dve", lambda e: e.reciprocal(out=rdn[:].rearrange("p a b -> p (a b)"), in_=pY[:, :]),
                     reads=[pYB], writes=[Brdn])
                S.op("dve", TTo(oT[:, :, tq], pX[:, :].rearrange("p (a b) -> p a b", a=4), rdn[:], ALU.mult),
                     reads=[pXB, Brdn], writes=[BoT[qb]])
                yield

            def units(kind, qb):
                gq = q0 // 128 + qb
                nk = (gq + 1) * 128
                if kind == "I":
                    return 8 * ((nk + 511) // 512)
                if kind == "B":
                    return 2 * NIT + 2
                return gq + 3

            def interleave(gens):
                live = [g for g in gens]
                while live:
                    live.sort(key=lambda g: g[2] / float(g[1]))
                    g = live[0]
                    try:
                        next(g[0])
                        g[2] += 1
                    except StopIteration:
                        live.remove(g)

            for step in range(nqb_h + 2):
                gens = []
                if step < nqb_h:
                    gens.append([stage_I(step), units("I", step), 0])
                if 0 <= step - 1 < nqb_h:
                    gens.append([stage_B(step - 1), units("B", step - 1), 0])
                if 0 <= step - 2 < nqb_h:
                    gens.append([stage_A(step - 2), units("A", step - 2), 0])
                interleave(gens)
            S.barrier()
            if stop < 5:
                continue

            woS = carve(MP, [8, D], BF16)
            BwoS = S.buf("woS")
            wov = wo_d.rearrange("(kc p) d -> p kc d", p=128)
            wbv = wb_d.rearrange("(g p) d -> p g d", p=128)
            for dc in range(8):
                cs = slice(dc * 128, (dc + 1) * 128)
                wta, wBa = wload(wa_d[:, :, cs], 4, 128)
                wtg, wBg = wload(wev[:, :, C_GA + dc * 128:C_GA + (dc + 1) * 128], 8, 128)
                wtb, wBb = wload(wbv[:, :, cs], 4, 128)
                wth, wBh = wload(wev[:, :, C_GBR + dc * 128:C_GBR + (dc + 1) * 128], 8, 128)
                if dc == 0:
                    for u in range(4):
                        S.dma("pool", woS[:, :, u * 256:(u + 1) * 256], wov[:, :, u * 256:(u + 1) * 256], sem_wo,
                              writes=[BwoS], hard=True)
                for tc in range(0, QH, CH):
                    ts_ = slice(tc, tc + CH)
                    oB = [BoT[tc // 128 + q] for q in range(CH // 128)]
                    pg, pgB = bank(0, 4)
                    for kc in range(8):
                        S.op("pe", MM(pg[:, 0:CH], wtg[:, kc, :], uT_full[:, kc, q0 + tc:q0 + tc + CH], kc == 0, kc == 7),
                             reads=[wBg] + xB(q0 + tc), writes=[pgB], signal=(kc == 7))
                    S.op("act", ACT(sgm[:], pg[:, 0:CH], AF.Sigmoid), reads=[pgB], writes=[Bsgm])
                    py, pyB = bank(0, 4)
                    for h4 in range(4):
                        S.op("pe", MM(py[:, 0:CH], wta[:, h4, :], oT[:, h4, ts_], h4 == 0, h4 == 3),
                             reads=[wBa] + oB, writes=[pyB], signal=(h4 == 3))
                    S.op("dve", TTo(tmpA[:], py[:, 0:CH], sgm[:], ALU.mult), reads=[pyB, Bsgm], writes=[BtmpA])
                    pg, pgB = bank(0, 4)
                    for kc in range(8):
                        S.op("pe", MM(pg[:, 0:CH], wth[:, kc, :], uT_full[:, kc, q0 + tc:q0 + tc + CH], kc == 0, kc == 7),
                             reads=[wBh] + xB(q0 + tc), writes=[pgB], signal=(kc == 7))
                    S.op("act", ACT(sgm[:], pg[:, 0:CH], AF.Sigmoid), reads=[pgB], writes=[Bsgm])
                    py, pyB = bank(0, 4)
                    for g in range(4):
                        S.op("pe", MM(py[:, 0:CH], wtb[:, g, :], zgT[:, g, ts_], g == 0, g == 3),
                             reads=[wBb, BzgT[tc // CH]], writes=[pyB], signal=(g == 3))
                    S.op("dve", TTo(tmpB[:], py[:, 0:CH], sgm[:], ALU.mult), reads=[pyB, Bsgm], writes=[BtmpB])
                    S.op("dve", TTo(mrgT[:, dc, ts_], tmpA[:], tmpB[:], ALU.add), reads=[BtmpA, BtmpB],
                         writes=[Bmrg[tc // CH]])
            for i in range(QH // 128):
                pa, paB = bank(4, 8)
                pb, pbB = bank(4, 8)
                for k, (pp, ppB) in enumerate(((pa, paB), (pb, pbB))):
                    for dc in range(8):
                        S.op("pe", MM(pp[:, :], mrgT[:, dc, i * 128:(i + 1) * 128], woS[:, dc, k * 512:(k + 1) * 512],
                                      dc == 0, dc == 7), reads=[Bmrg[(i * 128) // CH], BwoS], writes=[ppB], signal=(dc == 7))
                postnorm_res(pa[:, :], paB, pb[:, :], pbB, q0 // 128 + i, False)
            S.barrier()

    memT = carve(MP, [8, NMEM], BF16)
    kxT = carve(MP + 4096, [8, NMEM], BF16)
    vx = carve(MP + 8192, [2, D], BF16)
    qxT = carve(MP + 12288, [8, TT], BF16)
    oxT = carve(MP + 28672, [8, TT], BF16)
    PTx = [carve(MP + 45056, [2, CH], BF16), carve(MP + 47104, [2, CH], BF16)]
    rdx = carve(MP + 49152, [CH], F32)
    memst = carve(MP + 51200, [D], F32)
    xoS = carve(MP + 55296, [8, D], BF16)
    sem_wo = S.dmasem("wo")
    Brdn = S.buf("rdn")

    def xattn_mem(seq):
        BmemT = [S.buf("memT0"), S.buf("memT1")]
        Bmemst = S.buf("memst")
        for i in range(2):
            S.dma("sp", memst[:], mem_d[seq * NMEM + i * 128: seq * NMEM + (i + 1) * 128, :], sem_io, writes=[Bmemst])
            prenorm_T(lambda _i: memst[:], lambda _i: Bmemst, 2, memT, lambda _i, i=i: BmemT[i], i * 128, 1)
        BkxT, Bvx = S.buf("kxT"), S.buf("vx")
        xkv = xkv_d.rearrange("(kc p) c -> p kc c", p=128)

        def ev_k(ci, t, pt, pB, jn):
            S.op("act", ACT(kxT[:, ci, :], pt[:, 0:NMEM], AF.Copy), reads=[pB], writes=[BkxT])
        fm_linear(xkv, 0, D, memT, lambda t: BmemT, 0, NMEM, ev_k, CH=NMEM)
        for u in range(4):
            wt, wB = wload(xkv[:, :, D + u * 256:D + (u + 1) * 256], 8, 256)
            for mc in range(2):
                pt, pB = bank(0, 4)
                for kc in range(8):
                    S.op("pe", MM(pt[:, 0:256], memT[:, kc, mc * 128:(mc + 1) * 128], wt[:, kc, :], kc == 0, kc == 7),
                         reads=[wB, BmemT[mc]], writes=[pB], signal=(kc == 7))
                S.op("act", ACT(vx[:, mc, u * 256:(u + 1) * 256], pt[:, 0:256], AF.Copy), reads=[pB], writes=[Bvx])
        return BkxT, Bvx

    def xattn(seq, tt, BkxT, Bvx):
        uTf = carve(0, [8, TT], BF16)
        BqxT = [S.buf("qxT%d" % i) for i in range(TT // CH)]
        BoxT = [S.buf("oxT%d" % i) for i in range(TT // CH)]
        BPTx = [S.buf("PTx0"), S.buf("PTx1")]
        Brdx = S.buf("rdx")
        load_grow(2)
        BxoS = S.buf("xoS")
        xov = xo_d.rearrange("(kc p) d -> p kc d", p=128)
        for u in range(4):
            S.dma("pool", xoS[:, :, u * 256:(u + 1) * 256], xov[:, :, u * 256:(u + 1) * 256], sem_wo, writes=[BxoS], hard=True)
        prenorm_T(lambda i: h[:, tt * TPT + i, :], lambda i: hB[tt * TPT + i], 3, uTf, lambda i: uTB[i], 0, TPT)
        xqv = xq_d.rearrange("(kc p) c -> p kc c", p=128)

        def ev_q(ci, t, pt, pB, jn):
            S.op("act", ACT(qxT[:, ci, t:t + CH], pt[:, 0:CH], AF.Copy), reads=[pB], writes=[BqxT[t // CH]])
        fm_linear(xqv, 0, D, uTf, lambda t: [uTB[t // 128 + q] for q in range(CH // 128)], 0, TT, ev_q)
        ctr = 0
        for hd in range(4):
            for tc in range(0, TT, CH):
                P_, PB_ = PTx[ctr % 2], BPTx[ctr % 2]
                ctr += 1
                for mc in range(2):
                    pl, plB = bank(0, 4)
                    for dd in range(2):
                        S.op("pe", MM(pl[:, 0:CH], kxT[:, hd * 2 + dd, mc * 128:(mc + 1) * 128], qxT[:, hd * 2 + dd, tc:tc + CH],
                                      dd == 0, dd == 1), reads=[BkxT, BqxT[tc // CH]], writes=[plB], signal=(dd == 1))
                    S.op("act", ACT(P_[:, mc, :], pl[:, 0:CH], AF.Exp, scale=1.0 / 16.0), reads=[plB], writes=[PB_])
                pd, pdB = bank(0, 4)
                for mc in range(2):
                    S.op("pe", MM(pd[:, 0:CH], onesb[:, :], P_[:, mc, :], mc == 0, mc == 1), reads=[Bones, PB_],
                         writes=[pdB], signal=(mc == 1))
                S.op("dve", lambda e, pd=pd: e.reciprocal(out=rdx[:], in_=pd[:, 0:CH]), reads=[pdB], writes=[Brdx])
                for dd in range(2):
                    po, poB = bank(0, 4)
                    for mc in range(2):
                        S.op("pe", MM(po[:, 0:CH], vx[:, mc, (hd * 2 + dd) * 128:(hd * 2 + dd + 1) * 128], P_[:, mc, :],
                                      mc == 0, mc == 1), reads=[Bvx, PB_], writes=[poB], signal=(mc == 1))
                    S.op("dve", TTo(oxT[:, hd * 2 + dd, tc:tc + CH], po[:, 0:CH], rdx[:], ALU.mult), reads=[poB, Brdx],
                         writes=[BoxT[tc // CH]])
        for i in range(TPT):
            pa, paB = bank(4, 8)
            pb, pbB = bank(4, 8)
            for k, (pp, ppB) in enumerate(((pa, paB), (pb, pbB))):
                for dc in range(8):
                    S.op("pe", MM(pp[:, :], oxT[:, dc, i * 128:(i + 1) * 128], xoS[:, dc, k * 512:(k + 1) * 512],
                                  dc == 0, dc == 7), reads=[BoxT[(i * 128) // CH], BxoS], writes=[ppB], signal=(dc == 7))
            postnorm_res(pa[:, :], paB, pb[:, :], pbB, tt * TPT + i, False)
        S.barrier()

    def dump(k):
        if debug:
            for i in range(NT):
                S.dma("sp", dbg_d[k, i * 128:(i + 1) * 128, :], h[:, i, :], sem_out, reads=[hB[i]])

    for seq in range(NSEQ):
        for i in range(NT):
            S.dma("sp", h[:, i, :], x_d[seq * L + i * 128: seq * L + (i + 1) * 128, :], sem_io, writes=[hB[i]])
        for tt in range(NTT):
            if stop >= 2:
                ffn(seq, tt, 0, 0, 0)
            S.barrier()
        dump(0)
        if stop >= 3:
            load_grow(1)
            mixer(seq)
        dump(1)
        if stop >= 6:
            BkxT, Bvx = xattn_mem(seq)
            for tt in range(NTT):
                xattn(seq, tt, BkxT, Bvx)
        dump(2)
        for tt in range(NTT):
            if stop >= 7:
                ffn(seq, tt, 1, 4, 3)
            S.barrier()
        for i in range(NT):
            S.dma("sp", out_d[seq * L + i * 128: seq * L + (i + 1) * 128, :], h[:, i, :], sem_out, reads=[hB[i]])
    S.wait_all("sp", hB)
    S.barrier()
    S.emit()
    st.close()
    return nc


def _consts(L):
    cst = np.zeros((128, NCST), np.float32)
    cst[:, K_P2:K_P2 + 24] = (2.0 ** -np.arange(24, dtype=np.float64)).astype(np.float32)[None, :]
    t = np.arange(128)
    cst[:, K_TRI:K_TRI + 128] = np.where(t[None, :] <= t[:, None], 0.0, NEG).astype(np.float32)
    cst[:, K_ID:K_ID + 128] = np.eye(128, dtype=np.float32)
    pos = np.arange(L, dtype=np.float32)
    inv = (np.float32(10000.0) ** (-np.arange(0, 64, 2, dtype=np.float32) / np.float32(64))).astype(np.float32)
    ang = pos[:, None] * inv[None, :]
    c = np.cos(ang).astype(np.float32).T
    s = np.sin(ang).astype(np.float32).T
    c64 = np.concatenate([c, c], 0)
    s64 = np.concatenate([-s, s], 0)
    return cst, np.ascontiguousarray(np.concatenate([c64, c64], 0)), np.ascontiguousarray(np.concatenate([s64, s64], 0))


def host_prep(inp, L):
    g = lambda k: np.asarray(inp[k], np.float32)[0]
    cst, rc, rs = _consts(L)
    for gi, name in enumerate(["ffn1_pre_g", "mix_pre_g", "mem_g", "xattn_pre_g", "ffn2_pre_g"]):
        cst[:, K_G + gi * 8:K_G + gi * 8 + 8] = g(name).reshape(8, 128).T
    cst[:, K_CW:K_CW + 12] = g("conv_w").reshape(3, 4, 128).transpose(2, 1, 0).reshape(128, 12)
    grow = np.stack([np.broadcast_to(g(n)[None, :], (128, D)) for n in
                     ["ffn1_post_g", "mix_post_g", "xattn_post_g", "ffn2_post_g"]]).astype(np.float32)
    wa = g("w_a_out").reshape(4, 2, 64, D).transpose(1, 2, 0, 3).reshape(128, 4, D)
    shared = {
        "f1w1": g("ffn1_w1"), "f1w3": g("ffn1_w3"), "f1w2": g("ffn1_w2"),
        "f2w1": g("ffn2_w1"), "f2w3": g("ffn2_w3"), "f2w2": g("ffn2_w2"),
        "wext": np.ascontiguousarray(g("w_in")[:, _ext_cols()]),
        "wa": np.ascontiguousarray(wa), "wb": g("w_b_out"), "wo": g("w_out"),
        "xq": g("xattn_w_q"), "xkv": g("xattn_w_kv"), "xo": g("xattn_w_o"),
        "cst": cst, "grow": np.ascontiguousarray(grow), "ropec": rc, "ropes": rs,
    }
    return shared


_NC_CACHE = {}


def kernel(**inputs):
    x = np.asarray(inputs["x"], np.float32)
    mem = np.asarray(inputs["mem"], np.float32)
    B, L, _ = x.shape
    ncores = 8
    nseq = B // ncores
    key = (nseq, L)
    if key not in _NC_CACHE:
        _NC_CACHE[key] = build(nseq, L)
    nc = _NC_CACHE[key]
    shared = host_prep(inputs, L)
    in_maps = []
    for c in range(ncores):
        m = dict(shared)
        m["x"] = np.ascontiguousarray(x[c * nseq:(c + 1) * nseq].reshape(nseq * L, D))
        m["mem"] = np.ascontiguousarray(mem[c * nseq:(c + 1) * nseq].reshape(nseq * NMEM, D))
        in_maps.append(m)
    res = run_bass_kernel_spmd(nc, in_maps, core_ids=list(range(ncores)))
    outs = [np.asarray(r["out"], np.float32).reshape(nseq, L, D) for r in res.results]
    return np.concatenate(outs, axis=0)
```

```python
import numpy as np
from contextlib import ExitStack
import concourse.bass as bass
import concourse.mybir as mybir
from concourse.bass_utils import run_bass_kernel_spmd

F32 = mybir.dt.float32
BF16 = mybir.dt.bfloat16
AF = mybir.ActivationFunctionType
ALU = mybir.AluOpType
AX = mybir.AxisListType

D = 1024
DFF = 2816
NMEM = 256
EPS = 1e-6
NEG = -1.0e30
NIT = 20
EPOCH = 12000


class Buf:
    __slots__ = ("name", "lw", "rd")

    def __init__(self, name):
        self.name = name
        self.lw = None
        self.rd = []


class DmaSem:
    def __init__(self, S, name):
        self.key = ("dma", name)
        self.count = 0
        S.semkeys[self.key] = None


class Eng:
    def __init__(self, name):
        self.name = name
        self.count = 0
        self.epoch = 0
        self.ops = []
        self.known = {}


class Sched:
    def __init__(self, nc):
        self.nc = nc
        self.semkeys = {}
        self.eng = {n: Eng(n) for n in ("pe", "act", "dve", "pool", "sp")}
        for e in self.eng.values():
            self.semkeys[(e.name, 0)] = None
        self.dsems = {}
        self.nops = 0
        self.pool_deferred = []

    def buf(self, name):
        return Buf(name)

    def dmasem(self, name):
        d = DmaSem(self, name)
        self.dsems[d.key] = d
        return d

    def _deps(self, reads, writes):
        deps = []
        for b in reads:
            if b.lw is not None:
                deps.append(b.lw)
        for b in writes:
            if b.lw is not None:
                deps.append(b.lw)
            deps.extend(b.rd)
        return deps

    def _waits(self, e, deps):
        best = {}
        for key, v, snap in deps:
            if key[0] == "pe" and e.name == "pe":
                continue
            if key[0] == "dma":
                v = max(v, self.dsems[key].count)
            if e.known.get(key, 0) >= v:
                continue
            best[key] = max(best.get(key, 0), v)
            e.known[key] = v
            if snap is not None:
                for k2, v2 in snap.items():
                    if e.known.get(k2, 0) < v2:
                        e.known[k2] = v2
        return list(best.items())

    def _roll(self, e):
        if e.count >= EPOCH:
            e.epoch += 1
            e.count = 0
            self.semkeys[(e.name, e.epoch)] = None

    def op(self, eng, fn, reads=(), writes=(), signal=True):
        e = self.eng[eng]
        waits = self._waits(e, self._deps(reads, writes))
        self._roll(e)
        key = (e.name, e.epoch)
        if signal:
            e.count += 1
            tick = (key, e.count, dict(e.known))
            e.ops.append((waits, fn, key))
        else:
            tick = (key, e.count + 1, dict(e.known))
            e.ops.append((waits, fn, None))
        for b in reads:
            b.rd.append(tick)
        for b in writes:
            b.lw = tick
            b.rd = []
        self.nops += 1
        return tick

    def dma(self, eng, out, in_, sem, reads=(), writes=(), hard=False):
        e = self.eng[eng]
        waits = self._waits(e, self._deps(reads, writes))
        if hard and eng == "pool" and self.pool_deferred:
            for k, v in self.pool_deferred:
                if e.known.get(k, 0) < v:
                    waits.append((k, v))
                    e.known[k] = v
            self.pool_deferred = []
        sem.count += 16
        tick = (sem.key, sem.count, dict(e.known))

        def fn(engine, out=out, in_=in_):
            return engine.dma_start(out=out, in_=in_)
        e.ops.append((waits, fn, ("dmainc", sem.key)))
        for b in reads:
            b.rd.append(tick)
        for b in writes:
            b.lw = tick
            b.rd = []
        self.nops += 1
        return tick

    def wait_all(self, eng, bufs):
        e = self.eng[eng]
        waits = self._waits(e, self._deps((), bufs))
        e.ops.append((waits, None, None))

    def barrier(self):
        targets = []
        for e in self.eng.values():
            if e.count > 0:
                targets.append(((e.name, e.epoch), e.count))
        for key, d in self.dsems.items():
            if d.count > 0:
                targets.append((key, d.count))
        for e in self.eng.values():
            if e.name == "pool":
                self.pool_deferred = list(targets)
                continue
            waits = []
            for k, v in targets:
                if k[0] == "pe" and e.name == "pe":
                    continue
                if e.known.get(k, 0) < v:
                    waits.append((k, v))
                    e.known[k] = v
            e.ops.append((waits, None, None))

    def emit(self):
        nc = self.nc
        with ExitStack() as st:
            sems = {}
            for i, key in enumerate(self.semkeys):
                sems[key] = st.enter_context(nc.semaphore("s%d" % i))
            block = st.enter_context(nc.Block())

            def runner(e):
                def body(engine):
                    for waits, fn, key in e.ops:
                        for k, v in waits:
                            engine.wait_ge(sems[k], v)
                        if fn is None:
                            continue
                        ins = fn(engine)
                        if key is None:
                            continue
                        if key[0] == "dmainc":
                            ins.then_inc(sems[key[1]], 16)
                        else:
                            ins.then_inc(sems[key], 1)
                return body

            block.tensor(runner(self.eng["pe"]))
            block.scalar(runner(self.eng["act"]))
            block.vector(runner(self.eng["dve"]))
            block.gpsimd(runner(self.eng["pool"]))
            block.sync(runner(self.eng["sp"]))


def MM(out, lhsT, rhs, start, stop):
    return lambda e: e.matmul(out, lhsT, rhs, start=start, stop=stop)


def TR(out, in_, ident):
    return lambda e: e.transpose(out, in_, ident)


def ACT(out, in_, func, **kw):
    return lambda e: e.activation(out=out, in_=in_, func=func, **kw)


def TS(out, in0, s1, s2, op0, op1=None, **kw):
    if op1 is None:
        return lambda e: e.tensor_scalar(out=out, in0=in0, scalar1=s1, scalar2=s2, op0=op0, **kw)
    return lambda e: e.tensor_scalar(out=out, in0=in0, scalar1=s1, scalar2=s2, op0=op0, op1=op1, **kw)


def TTo(out, in0, in1, op):
    return lambda e: e.tensor_tensor(out=out, in0=in0, in1=in1, op=op)


def STT(out, in0, scalar, in1, op0, op1):
    return lambda e: e.scalar_tensor_tensor(out=out, in0=in0, scalar=scalar, in1=in1, op0=op0, op1=op1)


def CP(out, in_):
    return lambda e: e.tensor_copy(out=out, in_=in_)


def MS(ap, v):
    return lambda e: e.memset(ap, v)


_OFF = dict(qa=0, ka=512, va=576, qi=640, ki=1152, wi=1216, gb=1224, gc=1736, ci=2248, ga=2760, gbr=3784)


def _ext_cols():
    perm64 = np.concatenate([np.arange(32, 64), np.arange(0, 32)])
    cols = []
    qa = np.arange(0, 512)
    qi = np.arange(640, 1152)
    qap = (qa.reshape(8, 64)[:, perm64]).reshape(-1)
    qip = (qi.reshape(8, 64)[:, perm64]).reshape(-1)
    ka = np.arange(512, 576)
    ki = np.arange(1152, 1216)
    cols += [qa, qap, qi, qip]
    cols += [ka, ka, ka[perm64], ka[perm64], ki, ki, ki[perm64], ki[perm64]]
    vw = np.concatenate([np.arange(576, 640), np.arange(1216, 1224), np.full(56, 576)])
    cols += [vw]
    cols += [np.arange(1224, 1736), np.arange(1736, 2248), np.arange(2248, 2760)]
    cols += [np.arange(2760, 3784), np.arange(3784, 4808)]
    return np.concatenate(cols)


C_QA, C_QAP, C_QI, C_QIP = 0, 512, 1024, 1536
C_KA, C_KAP, C_KI, C_KIP = 2048, 2176, 2304, 2432
C_VW = 2560
C_GB, C_GC, C_CI = 2688, 3200, 3712
C_GA, C_GBR = 4224, 5248
C_EXT = 6272

K_G = 0
K_CW = 40
K_P2 = 52
K_TRI = 76
K_ID = 204
NCST = 332


def build(NSEQ, L, debug=False, stop=99):
    NT = L // 128
    TT = min(1024, L)
    NTT = L // TT
    CH = min(512, L)
    QH = TT
    NQH = L // QH
    KTOP = min(256, L // 4)
    TPT = TT // 128

    nc = bass.Bass("TRN2", target_bir_lowering=False)

    def din(name, shape, dt=F32):
        return nc.dram_tensor(name, list(shape), dt, kind="ExternalInput").ap()

    x_d = din("x", [NSEQ * L, D])
    mem_d = din("mem", [NSEQ * NMEM, D])
    out_d = nc.dram_tensor("out", [NSEQ * L, D], F32, kind="ExternalOutput").ap()
    w1_d = [din("f1w1", [D, DFF]), din("f2w1", [D, DFF])]
    w3_d = [din("f1w3", [D, DFF]), din("f2w3", [D, DFF])]
    w2_d = [din("f1w2", [DFF, D]), din("f2w2", [DFF, D])]
    wext_d = din("wext", [D, C_EXT])
    wa_d = din("wa", [128, 4, D])
    wb_d = din("wb", [512, D])
    wo_d = din("wo", [D, D])
    xq_d = din("xq", [D, D])
    xkv_d = din("xkv", [D, 2 * D])
    xo_d = din("xo", [D, D])
    cst_d = din("cst", [128, NCST])
    grow_d = din("grow", [4, 128, D])
    cos_d = din("ropec", [128, L])
    sin_d = din("ropes", [128, L])
    dbg_d = None
    if debug:
        dbg_d = nc.dram_tensor("dbg", [3, L, D], F32, kind="ExternalOutput").ap()

    S = Sched(nc)
    st = ExitStack()

    def sbuf(name, shape, dt):
        return st.enter_context(nc.sbuf_tensor("s_" + name, list(shape), dt))

    h = sbuf("h", [128, NT, D], F32)
    hB = [S.buf("h%d" % i) for i in range(NT)]
    AR_BYTES = 126976
    arena = sbuf("arena", [128, AR_BYTES // 2], BF16)
    cst = sbuf("cst", [128, NCST], F32)
    identb = sbuf("identb", [128, 128], BF16)
    onesb = sbuf("onesb", [128, 128], BF16)
    grow = sbuf("grow", [128, D], F32)
    stat = sbuf("stat", [128, 64], F32)
    bis = sbuf("bis", [128, 64], F32)
    junk = sbuf("junk", [128, 1024], BF16)
    hn = sbuf("hn", [128, 1024], BF16)
    Bhn = S.buf("hn")
    Bcst, Bid, Bones, Bgrow, Bjunk = (S.buf(n) for n in ("cst", "id", "ones", "grow", "junk"))
    Bjunk2 = S.buf("junk2")
    halo = sbuf("halo", [128, 8], F32)
    Bhalo = S.buf("halo")

    def carve(off, free_shape, dt):
        n = int(np.prod(free_shape))
        if dt == BF16:
            ap = arena[:, off // 2: off // 2 + n]
        else:
            ap = arena[:, off // 2: off // 2 + 2 * n].bitcast(F32)
        if len(free_shape) == 2:
            ap = ap.rearrange("p (a b) -> p a b", a=free_shape[0])
        elif len(free_shape) == 3:
            ap = ap.rearrange("p (a b c) -> p a b c", a=free_shape[0], b=free_shape[1])
        return ap

    POOL_OFF = 106496
    NSLOT = 5
    slots = []
    for i in range(NSLOT):
        slots.append(dict(off=POOL_OFF + i * 4096, buf=S.buf("slot%d" % i), sem=S.dmasem("slot%d" % i)))
    slot_ctr = [0]

    def wload(dview, a, b):
        assert a * b <= 2048, (a, b)
        sl = slots[slot_ctr[0] % NSLOT]
        slot_ctr[0] += 1
        t = carve(sl["off"], [a, b], BF16)
        S.dma("pool", t, dview, sl["sem"], writes=[sl["buf"]])
        return t, sl["buf"]

    psb = []
    for i in range(8):
        t = st.enter_context(nc.psum_tensor("ps%d" % i, [128, 512], F32))
        psb.append((t, S.buf("ps%d" % i)))
    rot = [0]

    def bank(lo=0, hi=4):
        i = lo + rot[0] % (hi - lo)
        rot[0] += 1
        return psb[i]

    sem_c = S.dmasem("const")
    S.dma("sp", cst[:], cst_d, sem_c, writes=[Bcst])
    S.dma("pool", identb[:], cst_d[:, K_ID:K_ID + 128], S.dmasem("const2"), writes=[Bid])
    S.op("dve", MS(onesb[:], 1.0), writes=[Bones])
    tri = cst[:, K_TRI:K_TRI + 128]
    identf = cst[:, K_ID:K_ID + 128]
    sem_g = S.dmasem("grow")
    sem_io = S.dmasem("io")
    sem_out = S.dmasem("out")
    sem_rope = S.dmasem("rope")

    stat_ctr = [0]
    Bstat = [S.buf("stat%d" % i) for i in range(16)]
    S.op("dve", MS(stat[:], 0.0), writes=Bstat)

    def stat_slot():
        i = stat_ctr[0] % 16
        stat_ctr[0] += 1
        return stat[:, 4 * i:4 * i + 4], Bstat[i]

    def rstd_from_ss(ss_ap, ssB, half):
        sl, slB = stat_slot()
        c = 4.0 if half else 1.0
        S.op("act", ACT(sl[:, 0:1], ss_ap, AF.Sqrt, scale=c / D, bias=c * EPS), reads=[ssB], writes=[slB])
        S.op("dve", lambda e: e.reciprocal(out=sl[:, 1:2], in_=sl[:, 0:1]), reads=[slB], writes=[slB])
        return sl[:, 1:2], slB

    def prenorm_T(src_tile, srcB, gidx, dst, dstB_of, ncols_dst0, ntiles):
        for i in range(ntiles):
            sl, slB = stat_slot()
            src = src_tile(i)
            S.op("act", ACT(junk[:], src, AF.Square, accum_out=sl[:, 2:3]), reads=[srcB(i)], writes=[Bjunk, slB])
            rs, rsB = rstd_from_ss(sl[:, 2:3], slB, False)
            S.op("act", ACT(hn[:], src, AF.Copy, scale=rs), reads=[srcB(i), rsB], writes=[Bhn])
            pt, pB = bank(0, 4)
            ptb = pt[:].bitcast(BF16)
            for dc in range(8):
                S.op("pe", TR(ptb[:, dc * 128:(dc + 1) * 128], hn[:, dc * 128:(dc + 1) * 128], identb[:]),
                     reads=[Bhn, Bid], writes=[pB], signal=(dc == 7))
            gv = cst[:, K_G + gidx * 8:K_G + gidx * 8 + 8]
            S.op("dve", TTo(dst[:, :, ncols_dst0 + i * 128: ncols_dst0 + (i + 1) * 128],
                            ptb.rearrange("p (a b) -> p a b", a=8),
                            gv.unsqueeze(2).to_broadcast([128, 8, 128]), ALU.mult),
                 reads=[pB, Bcst], writes=[dstB_of(i)])

    def postnorm_res(pa, paB, pb, pbB, ti, half):
        sl, slB = stat_slot()
        S.op("act", ACT(junk[:, 0:512], pa, AF.Square, accum_out=sl[:, 0:1]), reads=[paB], writes=[Bjunk, slB])
        S.op("act", ACT(junk[:, 512:1024], pb, AF.Square, accum_out=sl[:, 1:2]), reads=[pbB], writes=[Bjunk, slB])
        S.op("dve", TTo(sl[:, 2:3], sl[:, 0:1], sl[:, 1:2], ALU.add), reads=[slB], writes=[slB])
        rs, rsB = rstd_from_ss(sl[:, 2:3], slB, half)
        for k, (pp, ppB) in enumerate(((pa, paB), (pb, pbB))):
            hs = h[:, ti, k * 512:(k + 1) * 512]
            tmp = resid_tmp[k]
            S.op("dve", STT(tmp, pp, rs, grow[:, k * 512:(k + 1) * 512], ALU.mult, ALU.mult),
                 reads=[ppB, rsB, Bgrow], writes=[resid_B[k]])
            S.op("dve", TTo(hs, hs, tmp, ALU.add), reads=[resid_B[k], hB[ti]], writes=[hB[ti]])

    aux = sbuf("aux", [128, 2048], F32)
    resid_tmp = [aux[:, 1024:1536], aux[:, 1536:2048]]
    resid_B = [S.buf("resid0"), S.buf("resid1")]

    def load_grow(idx):
        S.dma("sp", grow[:], grow_d[idx], sem_g, writes=[Bgrow])

    uT_full = carve(0, [8, L], BF16)
    uTB = [S.buf("uT%d" % i) for i in range(NT)]

    def ffn(seq, tt, which, gidx_pre, grow_idx):
        t0 = tt * TT
        nch = TT // CH
        uTf = carve(0, [8, TT], BF16)
        gT = carve(16384, [22, TT], BF16)
        gTB = [S.buf("gT%d" % f) for f in range(22)]
        W2s = carve(61440, [22, D], BF16)
        W2B = S.buf("W2s")
        load_grow(grow_idx)
        prenorm_T(lambda i: h[:, tt * TPT + i, :], lambda i: hB[tt * TPT + i], gidx_pre,
                  uTf, lambda i: uTB[i], 0, TPT)
        w2v = w2_d[which].rearrange("(fc p) d -> p fc d", p=128)
        sem_w2 = sem_w2_list[0]
        w1v = w1_d[which].rearrange("(kc p) f -> p kc f", p=128)
        w3v = w3_d[which].rearrange("(kc p) f -> p kc f", p=128)
        for u in range(11):
            wt1, wb1 = wload(w1v[:, :, u * 256:(u + 1) * 256], 8, 256)
            wt3, wb3 = wload(w3v[:, :, u * 256:(u + 1) * 256], 8, 256)
            if u == 1:
                for f in range(0, 22, 2):
                    S.dma("pool", W2s[:, f:f + 2, :], w2v[:, f:f + 2, :], sem_w2, writes=[W2B], hard=True)
            for j in range(2):
                f = u * 2 + j
                for c in range(nch):
                    pg, pgB = bank(0, 4)
                    pu, puB = bank(0, 4)
                    rB = [uTB[(c * CH) // 128 + q] for q in range(CH // 128)]
                    for kc in range(8):
                        S.op("pe", MM(pg[:, 0:CH], wt1[:, kc, j * 128:(j + 1) * 128], uTf[:, kc, c * CH:(c + 1) * CH],
                                      kc == 0, kc == 7), reads=[wb1] + rB, writes=[pgB], signal=(kc == 7))
                    for kc in range(8):
                        S.op("pe", MM(pu[:, 0:CH], wt3[:, kc, j * 128:(j + 1) * 128], uTf[:, kc, c * CH:(c + 1) * CH],
                                      kc == 0, kc == 7), reads=[wb3] + rB, writes=[puB], signal=(kc == 7))
                    sg = silu_t[rot[0] % 2]
                    sgB = silu_B[rot[0] % 2]
                    S.op("act", ACT(sg[:, 0:CH], pg[:, 0:CH], AF.Silu), reads=[pgB], writes=[sgB])
                    S.op("dve", TTo(gT[:, f, c * CH:(c + 1) * CH], pu[:, 0:CH], sg[:, 0:CH], ALU.mult),
                         reads=[puB, sgB], writes=[gTB[f]])
        for i in range(TPT):
            pa, paB = bank(4, 8)
            pb, pbB = bank(4, 8)
            for k, (pp, ppB) in enumerate(((pa, paB), (pb, pbB))):
                for f in range(22):
                    S.op("pe", MM(pp[:, :], gT[:, f, i * 128:(i + 1) * 128], W2s[:, f, k * 512:(k + 1) * 512],
                                  f == 0, f == 21), reads=[gTB[f], W2B], writes=[ppB], signal=(f == 21))
            postnorm_res(pa[:, :], paB, pb[:, :], pbB, tt * TPT + i, True)

    sem_w2_list = [S.dmasem("w2")]
    silu_t = [aux[:, 0:512], aux[:, 512:1024]]
    silu_B = [S.buf("silu0"), S.buf("silu1")]

    def fm_linear(wv, c0, ncol, xT, xB_of_chunk, t0, ntok, evac, kcn=8, unit=256, CH=CH):
        unit = min(unit, 2048 // kcn)
        for u0 in range(0, ncol, unit):
            un = min(unit, ncol - u0)
            wt, wB = wload(wv[:, :, c0 + u0:c0 + u0 + un], kcn, un)
            for j in range(0, un, 128):
                jn = min(128, un - j)
                for tc in range(0, ntok, CH):
                    pt, pB = bank(0, 4)
                    for kc in range(kcn):
                        S.op("pe", MM(pt[0:jn, 0:CH], wt[:, kc, j:j + jn], xT[:, kc, t0 + tc:t0 + tc + CH],
                                      kc == 0, kc == kcn - 1),
                             reads=[wB] + xB_of_chunk(t0 + tc), writes=[pB], signal=(kc == kcn - 1))
                    evac((u0 + j) // 128, tc, pt, pB, jn)

    MP = 32768
    qaT = carve(MP, [4, QH], BF16)
    qiT = carve(MP + 8192, [4, QH], BF16)
    kaT = carve(MP + 16384, [L], BF16)
    kiT = carve(MP + 20480, [L], BF16)
    vS = carve(MP + 24576, [NT, 64], BF16)
    wiS = carve(MP + 26624, [NT, 8], F32)
    zgT = carve(MP + 27136, [4, QH], BF16)
    oT = carve(MP + 35328, [4, QH], BF16)
    RX = MP + 43520
    ropeC = carve(RX, [CH], F32)
    ropeS = carve(RX + 2048, [CH], F32)
    rt1 = carve(RX + 4096, [CH], F32)
    rt2 = carve(RX + 6144, [CH], F32)
    ybuf = carve(RX + 8192, [4, CH + 2], F32)
    ctmp = carve(RX + 8192 + 8224, [CH], F32)
    gcs = carve(RX + 8192 + 8224 + 2048, [CH], F32)
    sc = carve(RX, [L], F32)
    maskS = carve(RX + 8192, [L], BF16)
    maskT = carve(RX + 12288, [NT, 128], BF16)
    rh = [carve(RX + 16384, [512], F32), carve(RX + 18432, [512], F32)]
    PTs = [carve(RX + 20480 + i * 1024, [4, 128], BF16) for i in range(3)]
    rdn = carve(RX + 23552, [4, 128], F32)
    mask2 = carve(RX + 25600, [L], BF16)
    mrgT = carve(RX, [8, QH], BF16)
    sgm = carve(RX + 16384, [CH], F32)
    tmpA = carve(RX + 18432, [CH], F32)
    tmpB = carve(RX + 20480, [CH], F32)

    wev = wext_d.rearrange("(kc p) c -> p kc c", p=128)

    def mixer(seq):
        nqb_h = QH // 128
        BqaT = [S.buf("qaT%d" % i) for i in range(QH // CH)]
        BqiT = [S.buf("qiT%d" % i) for i in range(QH // CH)]
        BkaT = [S.buf("kaT%d" % i) for i in range(L // CH)]
        BkiT = [S.buf("kiT%d" % i) for i in range(L // CH)]
        BvS = [S.buf("vS%d" % i) for i in range(NT)]
        BwiS = [S.buf("wiS%d" % i) for i in range(NT)]
        BzgT = [S.buf("zgT%d" % i) for i in range(QH // CH)]
        BoT = [S.buf("oT%d" % i) for i in range(nqb_h)]
        BropeC, BropeS, Brt1, Brt2 = (S.buf(n) for n in ("ropeC", "ropeS", "rt1", "rt2"))
        Bybuf, Bctmp, Bgcs = (S.buf(n) for n in ("ybuf", "ctmp", "gcs"))
        Bsc, Bmask, BmaskT = S.buf("sc"), S.buf("mask"), S.buf("maskT")
        Brh = [S.buf("rh0"), S.buf("rh1")]
        BPT = [S.buf("PT%d" % i) for i in range(3)]
        Bmrg = [S.buf("mrg%d" % i) for i in range(QH // CH)]
        Bsgm, BtmpA, BtmpB = S.buf("sgm"), S.buf("tmpA"), S.buf("tmpB")
        Bbis = S.buf("bis")

        def xB(tcol):
            return [uTB[tcol // 128 + q] for q in range(CH // 128)]

        prenorm_T(lambda i: h[:, i, :], lambda i: hB[i], 1, uT_full, lambda i: uTB[i], 0, NT)

        def load_rope(tcol):
            S.dma("sp", ropeC[:], cos_d[:, tcol:tcol + CH], sem_rope, writes=[BropeC])
            S.dma("sp", ropeS[:], sin_d[:, tcol:tcol + CH], sem_rope, writes=[BropeS])

        def roped(c_plain, c_perm, ncol128, dst_of, dstB_of, t0, ntok):
            for g in range(ncol128):
                wt, wB = wload(wev[:, :, c_plain + g * 128:c_plain + (g + 1) * 128], 8, 128)
                wp, wpB = wload(wev[:, :, c_perm + g * 128:c_perm + (g + 1) * 128], 8, 128)
                for tc in range(0, ntok, CH):
                    p1, p1B = bank(0, 4)
                    p2, p2B = bank(0, 4)
                    for kc in range(8):
                        S.op("pe", MM(p1[:, 0:CH], wt[:, kc, :], uT_full[:, kc, t0 + tc:t0 + tc + CH], kc == 0, kc == 7),
                             reads=[wB] + xB(t0 + tc), writes=[p1B], signal=(kc == 7))
                    for kc in range(8):
                        S.op("pe", MM(p2[:, 0:CH], wp[:, kc, :], uT_full[:, kc, t0 + tc:t0 + tc + CH], kc == 0, kc == 7),
                             reads=[wpB] + xB(t0 + tc), writes=[p2B], signal=(kc == 7))
                    yield_rope(t0 + tc)
                    S.op("dve", TTo(rt1[:], p1[:, 0:CH], ropeC[:], ALU.mult), reads=[p1B, BropeC], writes=[Brt1])
                    S.op("dve", TTo(rt2[:], p2[:, 0:CH], ropeS[:], ALU.mult), reads=[p2B, BropeS], writes=[Brt2])
                    S.op("dve", TTo(dst_of(g, tc), rt1[:], rt2[:], ALU.add), reads=[Brt1, Brt2],
                         writes=[dstB_of(g, tc)])

        rope_state = [None]

        def yield_rope(tcol):
            if rope_state[0] != tcol:
                load_rope(tcol)
                rope_state[0] = tcol

        for qh in range(NQH):
            q0 = qh * QH
            rope_state[0] = None
            if qh == 0:
                for tc in range(0, L, CH):
                    roped(C_KA, C_KAP, 1, lambda g, t, tc=tc: kaT[:, tc:tc + CH], lambda g, t, tc=tc: BkaT[tc // CH], tc, CH)
                    roped(C_KI, C_KIP, 1, lambda g, t, tc=tc: kiT[:, tc:tc + CH], lambda g, t, tc=tc: BkiT[tc // CH], tc, CH)
                wt, wB = wload(wev[:, :, C_VW:C_VW + 128], 8, 128)
                for i in range(NT):
                    pt, pB = bank(0, 4)
                    for kc in range(8):
                        S.op("pe", MM(pt[:, 0:72], uT_full[:, kc, i * 128:(i + 1) * 128], wt[:, kc, 0:72], kc == 0, kc == 7),
                             reads=[wB, uTB[i]], writes=[pB], signal=(kc == 7))
                    S.op("act", ACT(vS[:, i, :], pt[:, 0:64], AF.Copy), reads=[pB], writes=[BvS[i]])
                    S.op("dve", TS(wiS[:, i, :], pt[:, 64:72], float(8 ** -0.5 * 64 ** -0.5), None, ALU.mult),
                         reads=[pB], writes=[BwiS[i]])
            for tc in range(0, QH, CH):
                roped(C_QA, C_QAP, 4, lambda g, t, tc=tc: qaT[:, g, tc:tc + CH], lambda g, t, tc=tc: BqaT[tc // CH], q0 + tc, CH)
                roped(C_QI, C_QIP, 4, lambda g, t, tc=tc: qiT[:, g, tc:tc + CH], lambda g, t, tc=tc: BqiT[tc // CH], q0 + tc, CH)
            cw = cst[:, K_CW:K_CW + 12].rearrange("p (g k) -> p g k", g=4)
            for tc in range(0, QH, CH):
                first = (q0 + tc == 0)
                for g in range(4):
                    if first:
                        S.op("dve", MS(ybuf[:, g, 0:2], 0.0), writes=[Bybuf])
                    elif tc == 0:
                        S.op("dve", CP(ybuf[:, g, 0:2], halo[:, 2 * g:2 * g + 2]), reads=[Bhalo], writes=[Bybuf])
                    else:
                        S.op("dve", CP(ybuf[:, g, 0:2], ybuf[:, g, CH:CH + 2]), reads=[Bybuf], writes=[Bybuf])

                    def ev_gc(ci, t, pt, pB, jn):
                        S.op("act", ACT(gcs[:], pt[:, 0:CH], AF.Copy), reads=[pB], writes=[Bgcs])
                    fm_linear(wev, C_GC + g * 128, 128, uT_full, xB, q0 + tc, CH, ev_gc, unit=128)

                    def ev_ci(ci, t, pt, pB, jn, g=g):
                        S.op("dve", TTo(ybuf[:, g, 2:CH + 2], pt[:, 0:CH], gcs[:], ALU.mult), reads=[pB, Bgcs], writes=[Bybuf])
                        S.op("dve", TS(ctmp[:], ybuf[:, g, 2:CH + 2], cw[:, g, 2:3], None, ALU.mult),
                             reads=[Bybuf, Bcst], writes=[Bctmp])
                        S.op("dve", STT(ctmp[:], ybuf[:, g, 1:CH + 1], cw[:, g, 1:2], ctmp[:], ALU.mult, ALU.add),
                             reads=[Bybuf, Bcst, Bctmp], writes=[Bctmp])
                        S.op("dve", STT(ctmp[:], ybuf[:, g, 0:CH], cw[:, g, 0:1], ctmp[:], ALU.mult, ALU.add),
                             reads=[Bybuf, Bcst, Bctmp], writes=[Bctmp])
                    fm_linear(wev, C_CI + g * 128, 128, uT_full, xB, q0 + tc, CH, ev_ci, unit=128)

                    def ev_gb(ci, t, pt, pB, jn, g=g, tc=tc):
                        S.op("dve", TTo(zgT[:, g, tc:tc + CH], pt[:, 0:CH], ctmp[:], ALU.mult), reads=[pB, Bctmp],
                             writes=[BzgT[tc // CH]])
                    fm_linear(wev, C_GB + g * 128, 128, uT_full, xB, q0 + tc, CH, ev_gb, unit=128)
                    if tc + CH == QH and qh + 1 < NQH:
                        S.op("dve", CP(halo[:, 2 * g:2 * g + 2], ybuf[:, g, CH:CH + 2]), reads=[Bybuf], writes=[Bhalo])
            S.barrier()
            if stop < 4:
                continue

            p2 = cst[:, K_P2:K_P2 + NIT + 2]
            scs = [sc, aux[:, 0:L]]
            Bscs = [S.buf("sc0"), S.buf("sc1")]
            masks = [maskS, mask2]
            BmL = [S.buf("mL0"), S.buf("mL1")]
            BmR = [S.buf("mR0"), S.buf("mR1")]
            Bb = [dict((n, S.buf("b%s%d" % (n, p))) for n in ("mid", "cd", "sa", "tot", "tmp", "wt")) for p in range(2)]

            def stage_I(qb):
                gq = q0 // 128 + qb
                nk = (gq + 1) * 128
                tq = slice(qb * 128, (qb + 1) * 128)
                p = qb % 2
                for k0 in range(0, nk, 512):
                    kw = min(512, nk - k0)
                    pacc, paccB = psb[2]
                    for hh in range(8):
                        pr, prB = psb[hh % 2]
                        pb_ = (hh % 2) * 64
                        S.op("pe", MM(pr[:, 0:kw], qiT[pb_:pb_ + 64, hh // 2, tq], kiT[pb_:pb_ + 64, k0:k0 + kw], True, True),
                             reads=[BqiT[(qb * 128) // CH], BkiT[k0 // CH], BkiT[(k0 + kw - 1) // CH]], writes=[prB])
                        r, rB = rh[hh % 2], Brh[hh % 2]
                        wcol = wiS[:, gq, hh:hh + 1]
                        if hh == 0:
                            S.op("dve", TS(pacc[:, 0:kw], pr[:, 0:kw], 0.0, wcol, ALU.max, ALU.mult), reads=[prB, BwiS[gq]],
                                 writes=[paccB])
                        else:
                            S.op("dve", TS(r[:, 0:kw], pr[:, 0:kw], 0.0, wcol, ALU.max, ALU.mult), reads=[prB, BwiS[gq]],
                                 writes=[rB])
                            S.op("dve", TTo(pacc[:, 0:kw], r[:, 0:kw], pacc[:, 0:kw], ALU.add), reads=[rB, paccB],
                                 writes=[paccB])
                        yield
                    S.op("act", ACT(scs[p][:, k0:k0 + kw], pacc[:, 0:kw], AF.Copy), reads=[paccB], writes=[Bscs[p]])

            def stage_B(qb):
                gq = q0 // 128 + qb
                nk = (gq + 1) * 128
                p = qb % 2
                sc_, Bsc_ = scs[p], Bscs[p]
                mk = masks[p]
                bb = Bb[p]
                o = 32 * p
                cA, cmid, ccd, csa, ctot, ctmp_, cthr, cW = (bis[:, o + i:o + i + 1] for i in range(8))
                wt_ = bis[:, o + 8:o + 8 + NIT + 2]
                csg = ctot
                cbias = float(nk - 2 * KTOP + 1)
                S.op("dve", lambda e: e.tensor_reduce(out=cA, in_=sc_[:, 0:nk], axis=AX.X, op=ALU.max,
                                                       apply_absolute_value=True), reads=[Bsc_], writes=[bb["wt"]])
                S.op("dve", TS(cW, cA, 2.000002, 1.0e-20, ALU.mult, ALU.add), reads=[bb["wt"]], writes=[bb["wt"]])
                S.op("dve", TS(wt_, p2, cW, None, ALU.mult), reads=[bb["wt"], Bcst], writes=[bb["wt"]])
                S.op("dve", TTo(sc_[:, nk - 128:nk], sc_[:, nk - 128:nk], tri, ALU.add), reads=[Bsc_, Bcst], writes=[Bsc_])
                S.op("dve", MS(cmid, 0.0), writes=[bb["mid"]])
                yield
                for it in range(1, NIT + 1):
                    S.op("act", ACT(mk[:, 0:nk], sc_[:, 0:nk], AF.Sign, scale=-1.0, bias=cmid, accum_out=csa),
                         reads=[Bsc_, bb["mid"]], writes=[BmL[p], BmR[p], bb["sa"]])
                    S.op("act", ACT(csg, csa, AF.Sign, scale=-1.0, bias=cbias), reads=[bb["sa"]], writes=[bb["tot"]])
                    S.op("act", ACT(cmid, csg, AF.Identity, scale=wt_[:, it + 1:it + 2], bias=cmid),
                         reads=[bb["tot"], bb["wt"], bb["mid"]], writes=[bb["mid"]])
                    yield
                S.op("dve", TS(cthr, cmid, wt_[:, NIT + 1:NIT + 2], None, ALU.subtract), reads=[bb["mid"], bb["wt"]],
                     writes=[bb["tmp"]])
                S.op("dve", TS(mk[:, 0:nk], sc_[:, 0:nk], cthr, None, ALU.is_ge), reads=[Bsc_, bb["tmp"]],
                     writes=[BmL[p], BmR[p]])
                yield

            def stage_A(qb):
                gq = q0 // 128 + qb
                tq = slice(qb * 128, (qb + 1) * 128)
                p = qb % 2
                mk = masks[p]
                pm, pmB = psb[3]
                pmb = pm[:].bitcast(BF16)
                for j0 in range(0, gq + 1, 8):
                    jn = min(8, gq + 1 - j0)
                    for j in range(jn):
                        S.op("pe", TR(pmb[:, j * 128:(j + 1) * 128], mk[:, (j0 + j) * 128:(j0 + j + 1) * 128], identb[:]),
                             reads=[BmL[p], BmR[p], Bid], writes=[pmB], signal=(j == jn - 1))
                    S.op("act", ACT(maskT[:, j0:j0 + jn, :], pmb[:, 0:jn * 128].rearrange("p (a b) -> p a b", a=jn), AF.Copy),
                         reads=[pmB], writes=[BmaskT])
                yield
                pX, pXB = psb[6]
                pY, pYB = psb[7]
                ctr = 0
                for j in range(gq + 1):
                    ks = slice(j * 128, (j + 1) * 128)
                    for pair in range(4):
                        for par in range(2):
                            pl, plB = psb[4 + par]
                            pb_ = par * 64
                            S.op("pe", MM(pl[:, pair * 128:(pair + 1) * 128], kaT[pb_:pb_ + 64, ks], qaT[pb_:pb_ + 64, pair, tq],
                                          True, True), reads=[BkaT[(j * 128) // CH], BqaT[(qb * 128) // CH]], writes=[plB],
                                 signal=(pair == 3 and par == 1))
                    for par in range(2):
                        pl, plB = psb[4 + par]
                        PT, PTB = PTs[ctr % 3], BPT[ctr % 3]
                        ctr += 1
                        S.op("act", ACT(PT[:], pl[:].rearrange("p (a b) -> p a b", a=4), AF.Exp, scale=0.125),
                             reads=[plB], writes=[PTB])
                        S.op("dve", TTo(PT[:], PT[:], maskT[:, j, :].unsqueeze(1).to_broadcast([128, 4, 128]), ALU.mult),
                             reads=[PTB, BmaskT], writes=[PTB])
                        ptf = PT[:].rearrange("p a b -> p (a b)")
                        S.op("pe", MM(pX[par * 64:(par + 1) * 64, :], vS[:, j, :], ptf, j == 0, j == gq),
                             reads=[BvS[j], PTB], writes=[pXB], signal=False)
                        S.op("pe", MM(pY[par * 64:(par + 1) * 64, :], onesb[:, 0:64], ptf, j == 0, j == gq),
                             reads=[Bones, PTB], writes=[pYB], signal=True)
                    yield
                S.op("dve", lambda e: e.reciprocal(out=rdn[:].rearrange("p a b -> p (a b)"), in_=pY[:, :]),
                     reads=[pYB], writes=[Brdn])
                S.op("dve", TTo(oT[:, :, tq], pX[:, :].rearrange("p (a b) -> p a b", a=4), rdn[:], ALU.mult),
                     reads=[pXB, Brdn], writes=[BoT[qb]])
                yield

            def units(kind, qb):
                gq = q0 // 128 + qb
                nk = (gq + 1) * 128
                if kind == "I":
                    return 8 * ((nk + 511) // 512)
                if kind == "B":
                    return NIT + 2
                return gq + 3

            def interleave(gens):
                live = [g for g in gens]
                while live:
                    live.sort(key=lambda g: g[2] / float(g[1]))
                    g = live[0]
                    try:
                        next(g[0])
                        g[2] += 1
                    except StopIteration:
                        live.remove(g)

            for step in range(nqb_h + 2):
                gens = []
                if step < nqb_h:
                    gens.append([stage_I(step), units("I", step), 0])
                if 0 <= step - 1 < nqb_h:
                    gens.append([stage_B(step - 1), units("B", step - 1), 0])
                if 0 <= step - 2 < nqb_h:
                    gens.append([stage_A(step - 2), units("A", step - 2), 0])
                interleave(gens)
            S.barrier()
            if stop < 5:
                continue

            woS = carve(MP, [8, D], BF16)
            BwoS = S.buf("woS")
            wov = wo_d.rearrange("(kc p) d -> p kc d", p=128)
            wbv = wb_d.rearrange("(g p) d -> p g d", p=128)
            for dc in range(8):
                cs = slice(dc * 128, (dc + 1) * 128)
                wta, wBa = wload(wa_d[:, :, cs], 4, 128)
                wtg, wBg = wload(wev[:, :, C_GA + dc * 128:C_GA + (dc + 1) * 128], 8, 128)
                wtb, wBb = wload(wbv[:, :, cs], 4, 128)
                wth, wBh = wload(wev[:, :, C_GBR + dc * 128:C_GBR + (dc + 1) * 128], 8, 128)
                if dc == 0:
                    for u in range(4):
                        S.dma("pool", woS[:, :, u * 256:(u + 1) * 256], wov[:, :, u * 256:(u + 1) * 256], sem_wo,
                              writes=[BwoS], hard=True)
                for tc in range(0, QH, CH):
                    ts_ = slice(tc, tc + CH)
                    oB = [BoT[tc // 128 + q] for q in range(CH // 128)]
                    pg, pgB = bank(0, 4)
                    for kc in range(8):
                        S.op("pe", MM(pg[:, 0:CH], wtg[:, kc, :], uT_full[:, kc, q0 + tc:q0 + tc + CH], kc == 0, kc == 7),
                             reads=[wBg] + xB(q0 + tc), writes=[pgB], signal=(kc == 7))
                    S.op("act", ACT(sgm[:], pg[:, 0:CH], AF.Sigmoid), reads=[pgB], writes=[Bsgm])
                    py, pyB = bank(0, 4)
                    for h4 in range(4):
                        S.op("pe", MM(py[:, 0:CH], wta[:, h4, :], oT[:, h4, ts_], h4 == 0, h4 == 3),
                             reads=[wBa] + oB, writes=[pyB], signal=(h4 == 3))
                    S.op("dve", TTo(tmpA[:], py[:, 0:CH], sgm[:], ALU.mult), reads=[pyB, Bsgm], writes=[BtmpA])
                    pg, pgB = bank(0, 4)
                    for kc in range(8):
                        S.op("pe", MM(pg[:, 0:CH], wth[:, kc, :], uT_full[:, kc, q0 + tc:q0 + tc + CH], kc == 0, kc == 7),
                             reads=[wBh] + xB(q0 + tc), writes=[pgB], signal=(kc == 7))
                    S.op("act", ACT(sgm[:], pg[:, 0:CH], AF.Sigmoid), reads=[pgB], writes=[Bsgm])
                    py, pyB = bank(0, 4)
                    for g in range(4):
                        S.op("pe", MM(py[:, 0:CH], wtb[:, g, :], zgT[:, g, ts_], g == 0, g == 3),
                             reads=[wBb, BzgT[tc // CH]], writes=[pyB], signal=(g == 3))
                    S.op("dve", TTo(tmpB[:], py[:, 0:CH], sgm[:], ALU.mult), reads=[pyB, Bsgm], writes=[BtmpB])
                    S.op("dve", TTo(mrgT[:, dc, ts_], tmpA[:], tmpB[:], ALU.add), reads=[BtmpA, BtmpB],
                         writes=[Bmrg[tc // CH]])
            for i in range(QH // 128):
                pa, paB = bank(4, 8)
                pb, pbB = bank(4, 8)
                for k, (pp, ppB) in enumerate(((pa, paB), (pb, pbB))):
                    for dc in range(8):
                        S.op("pe", MM(pp[:, :], mrgT[:, dc, i * 128:(i + 1) * 128], woS[:, dc, k * 512:(k + 1) * 512],
                                      dc == 0, dc == 7), reads=[Bmrg[(i * 128) // CH], BwoS], writes=[ppB], signal=(dc == 7))
                postnorm_res(pa[:, :], paB, pb[:, :], pbB, q0 // 128 + i, False)
            S.barrier()

    memT = carve(MP, [8, NMEM], BF16)
    kxT = carve(MP + 4096, [8, NMEM], BF16)
    vx = carve(MP + 8192, [2, D], BF16)
    qxT = carve(MP + 12288, [8, TT], BF16)
    oxT = carve(MP + 28672, [8, TT], BF16)
    PTx = [carve(MP + 45056, [2, CH], BF16), carve(MP + 47104, [2, CH], BF16)]
    rdx = carve(MP + 49152, [CH], F32)
    memst = carve(MP + 51200, [D], F32)
    xoS = carve(MP + 55296, [8, D], BF16)
    sem_wo = S.dmasem("wo")
    Brdn = S.buf("rdn")

    def xattn_mem(seq):
        BmemT = [S.buf("memT0"), S.buf("memT1")]
        Bmemst = S.buf("memst")
        for i in range(2):
            S.dma("sp", memst[:], mem_d[seq * NMEM + i * 128: seq * NMEM + (i + 1) * 128, :], sem_io, writes=[Bmemst])
            prenorm_T(lambda _i: memst[:], lambda _i: Bmemst, 2, memT, lambda _i, i=i: BmemT[i], i * 128, 1)
        BkxT, Bvx = S.buf("kxT"), S.buf("vx")
        xkv = xkv_d.rearrange("(kc p) c -> p kc c", p=128)

        def ev_k(ci, t, pt, pB, jn):
            S.op("act", ACT(kxT[:, ci, :], pt[:, 0:NMEM], AF.Copy), reads=[pB], writes=[BkxT])
        fm_linear(xkv, 0, D, memT, lambda t: BmemT, 0, NMEM, ev_k, CH=NMEM)
        for u in range(4):
            wt, wB = wload(xkv[:, :, D + u * 256:D + (u + 1) * 256], 8, 256)
            for mc in range(2):
                pt, pB = bank(0, 4)
                for kc in range(8):
                    S.op("pe", MM(pt[:, 0:256], memT[:, kc, mc * 128:(mc + 1) * 128], wt[:, kc, :], kc == 0, kc == 7),
                         reads=[wB, BmemT[mc]], writes=[pB], signal=(kc == 7))
                S.op("act", ACT(vx[:, mc, u * 256:(u + 1) * 256], pt[:, 0:256], AF.Copy), reads=[pB], writes=[Bvx])
        return BkxT, Bvx

    def xattn(seq, tt, BkxT, Bvx):
        uTf = carve(0, [8, TT], BF16)
        BqxT = [S.buf("qxT%d" % i) for i in range(TT // CH)]
        BoxT = [S.buf("oxT%d" % i) for i in range(TT // CH)]
        BPTx = [S.buf("PTx0"), S.buf("PTx1")]
        Brdx = S.buf("rdx")
        load_grow(2)
        BxoS = S.buf("xoS")
        xov = xo_d.rearrange("(kc p) d -> p kc d", p=128)
        for u in range(4):
            S.dma("pool", xoS[:, :, u * 256:(u + 1) * 256], xov[:, :, u * 256:(u + 1) * 256], sem_wo, writes=[BxoS], hard=True)
        prenorm_T(lambda i: h[:, tt * TPT + i, :], lambda i: hB[tt * TPT + i], 3, uTf, lambda i: uTB[i], 0, TPT)
        xqv = xq_d.rearrange("(kc p) c -> p kc c", p=128)

        def ev_q(ci, t, pt, pB, jn):
            S.op("act", ACT(qxT[:, ci, t:t + CH], pt[:, 0:CH], AF.Copy), reads=[pB], writes=[BqxT[t // CH]])
        fm_linear(xqv, 0, D, uTf, lambda t: [uTB[t // 128 + q] for q in range(CH // 128)], 0, TT, ev_q)
        ctr = 0
        for hd in range(4):
            for tc in range(0, TT, CH):
                P_, PB_ = PTx[ctr % 2], BPTx[ctr % 2]
                ctr += 1
                for mc in range(2):
                    pl, plB = bank(0, 4)
                    for dd in range(2):
                        S.op("pe", MM(pl[:, 0:CH], kxT[:, hd * 2 + dd, mc * 128:(mc + 1) * 128], qxT[:, hd * 2 + dd, tc:tc + CH],
                                      dd == 0, dd == 1), reads=[BkxT, BqxT[tc // CH]], writes=[plB], signal=(dd == 1))
                    S.op("act", ACT(P_[:, mc, :], pl[:, 0:CH], AF.Exp, scale=1.0 / 16.0), reads=[plB], writes=[PB_])
                pd, pdB = bank(0, 4)
                for mc in range(2):
                    S.op("pe", MM(pd[:, 0:CH], onesb[:, :], P_[:, mc, :], mc == 0, mc == 1), reads=[Bones, PB_],
                         writes=[pdB], signal=(mc == 1))
                S.op("dve", lambda e, pd=pd: e.reciprocal(out=rdx[:], in_=pd[:, 0:CH]), reads=[pdB], writes=[Brdx])
                for dd in range(2):
                    po, poB = bank(0, 4)
                    for mc in range(2):
                        S.op("pe", MM(po[:, 0:CH], vx[:, mc, (hd * 2 + dd) * 128:(hd * 2 + dd + 1) * 128], P_[:, mc, :],
                                      mc == 0, mc == 1), reads=[Bvx, PB_], writes=[poB], signal=(mc == 1))
                    S.op("dve", TTo(oxT[:, hd * 2 + dd, tc:tc + CH], po[:, 0:CH], rdx[:], ALU.mult), reads=[poB, Brdx],
                         writes=[BoxT[tc // CH]])
        for i in range(TPT):
            pa, paB = bank(4, 8)
            pb, pbB = bank(4, 8)
            for k, (pp, ppB) in enumerate(((pa, paB), (pb, pbB))):
                for dc in range(8):
                    S.op("pe", MM(pp[:, :], oxT[:, dc, i * 128:(i + 1) * 128], xoS[:, dc, k * 512:(k + 1) * 512],
                                  dc == 0, dc == 7), reads=[BoxT[(i * 128) // CH], BxoS], writes=[ppB], signal=(dc == 7))
            postnorm_res(pa[:, :], paB, pb[:, :], pbB, tt * TPT + i, False)
        S.barrier()

    def dump(k):
        if debug:
            for i in range(NT):
                S.dma("sp", dbg_d[k, i * 128:(i + 1) * 128, :], h[:, i, :], sem_out, reads=[hB[i]])

    for seq in range(NSEQ):
        for i in range(NT):
            S.dma("sp", h[:, i, :], x_d[seq * L + i * 128: seq * L + (i + 1) * 128, :], sem_io, writes=[hB[i]])
        for tt in range(NTT):
            if stop >= 2:
                ffn(seq, tt, 0, 0, 0)
            S.barrier()
        dump(0)
        if stop >= 3:
            load_grow(1)
            mixer(seq)
        dump(1)
        if stop >= 6:
            BkxT, Bvx = xattn_mem(seq)
            for tt in range(NTT):
                xattn(seq, tt, BkxT, Bvx)
        dump(2)
        for tt in range(NTT):
            if stop >= 7:
                ffn(seq, tt, 1, 4, 3)
            S.barrier()
        for i in range(NT):
            S.dma("sp", out_d[seq * L + i * 128: seq * L + (i + 1) * 128, :], h[:, i, :], sem_out, reads=[hB[i]])
    S.wait_all("sp", hB)
    S.barrier()
    S.emit()
    st.close()
    return nc


def _consts(L):
    cst = np.zeros((128, NCST), np.float32)
    cst[:, K_P2:K_P2 + 24] = (2.0 ** -np.arange(24, dtype=np.float64)).astype(np.float32)[None, :]
    t = np.arange(128)
    cst[:, K_TRI:K_TRI + 128] = np.where(t[None, :] <= t[:, None], 0.0, NEG).astype(np.float32)
    cst[:, K_ID:K_ID + 128] = np.eye(128, dtype=np.float32)
    pos = np.arange(L, dtype=np.float32)
    inv = (np.float32(10000.0) ** (-np.arange(0, 64, 2, dtype=np.float32) / np.float32(64))).astype(np.float32)
    ang = pos[:, None] * inv[None, :]
    c = np.cos(ang).astype(np.float32).T
    s = np.sin(ang).astype(np.float32).T
    c64 = np.concatenate([c, c], 0)
    s64 = np.concatenate([-s, s], 0)
    return cst, np.ascontiguousarray(np.concatenate([c64, c64], 0)), np.ascontiguousarray(np.concatenate([s64, s64], 0))


def host_prep(inp, L):
    g = lambda k: np.asarray(inp[k], np.float32)[0]
    cst, rc, rs = _consts(L)
    for gi, name in enumerate(["ffn1_pre_g", "mix_pre_g", "mem_g", "xattn_pre_g", "ffn2_pre_g"]):
        cst[:, K_G + gi * 8:K_G + gi * 8 + 8] = g(name).reshape(8, 128).T
    cst[:, K_CW:K_CW + 12] = g("conv_w").reshape(3, 4, 128).transpose(2, 1, 0).reshape(128, 12)
    grow = np.stack([np.broadcast_to(g(n)[None, :], (128, D)) for n in
                     ["ffn1_post_g", "mix_post_g", "xattn_post_g", "ffn2_post_g"]]).astype(np.float32)
    wa = g("w_a_out").reshape(4, 2, 64, D).transpose(1, 2, 0, 3).reshape(128, 4, D)
    shared = {
        "f1w1": g("ffn1_w1"), "f1w3": g("ffn1_w3"), "f1w2": g("ffn1_w2"),
        "f2w1": g("ffn2_w1"), "f2w3": g("ffn2_w3"), "f2w2": g("ffn2_w2"),
        "wext": np.ascontiguousarray(g("w_in")[:, _ext_cols()]),
        "wa": np.ascontiguousarray(wa), "wb": g("w_b_out"), "wo": g("w_out"),
        "xq": g("xattn_w_q"), "xkv": g("xattn_w_kv"), "xo": g("xattn_w_o"),
        "cst": cst, "grow": np.ascontiguousarray(grow), "ropec": rc, "ropes": rs,
    }
    return shared


_NC_CACHE = {}


def kernel(**inputs):
    x = np.asarray(inputs["x"], np.float32)
    mem = np.asarray(inputs["mem"], np.float32)
    B, L, _ = x.shape
    ncores = 8
    nseq = B // ncores
    key = (nseq, L)
    if key not in _NC_CACHE:
        _NC_CACHE[key] = build(nseq, L)
    nc = _NC_CACHE[key]
    shared = host_prep(inputs, L)
    in_maps = []
    for c in range(ncores):
        m = dict(shared)
        m["x"] = np.ascontiguousarray(x[c * nseq:(c + 1) * nseq].reshape(nseq * L, D))
        m["mem"] = np.ascontiguousarray(mem[c * nseq:(c + 1) * nseq].reshape(nseq * NMEM, D))
        in_maps.append(m)
    res = run_bass_kernel_spmd(nc, in_maps, core_ids=list(range(ncores)))
    outs = [np.asarray(r["out"], np.float32).reshape(nseq, L, D) for r in res.results]
    return np.concatenate(outs, axis=0)
```
